# Optimizing a Trainium2 kernel written in Bass

```python
import math
import jax
import jax.numpy as jnp
from jax import lax
import numpy as np

D_MODEL = 1024
BATCH = 32
SEQ = 2048
DEPTH = 1

N_META = 16
BLOCK = 128
PAD = BLOCK - N_META
WINDOW = 128
A_HEADS = 8
A_KV_HEADS = 2
A_HEAD_DIM = 64
B_HEADS = 8
B_HEAD_DIM = 64
B_Q_RANK = 256
B_KV_RANK = 128
IDX_HEADS = 4
IDX_DIM = 64
TOPK_MAX = 256
N_BUCKETS = 32
MAX_DISTANCE = 128
TOTAL_HEADS = A_HEADS + B_HEADS
D_FF = -(-8 * D_MODEL // (3 * 256)) * 256
EPS = 1e-6
NEG = -1e30

IN_WIDTHS = (A_HEADS * A_HEAD_DIM, A_KV_HEADS * A_HEAD_DIM, A_KV_HEADS * A_HEAD_DIM,
             B_Q_RANK, B_KV_RANK, IDX_HEADS * IDX_DIM, IDX_DIM, IDX_HEADS, 2 * D_MODEL)
IN_SPLITS = tuple(sum(IN_WIDTHS[:i + 1]) for i in range(len(IN_WIDTHS) - 1))
D_IN = sum(IN_WIDTHS)

kernel_name = "hybrid_gated_swa_sink_dsa_mla_block"


def rms_norm(x, g):
    xf = x.astype(jnp.float32)
    y = xf * lax.rsqrt(jnp.mean(xf * xf, axis=-1, keepdims=True) + EPS)
    return (y * g.astype(jnp.float32)).astype(x.dtype)


def layer_norm(x, g, b):
    xf = x.astype(jnp.float32)
    mu = jnp.mean(xf, axis=-1, keepdims=True)
    xc = xf - mu
    var = jnp.mean(xc * xc, axis=-1, keepdims=True)
    return (xc * lax.rsqrt(var + EPS) * g.astype(jnp.float32) + b.astype(jnp.float32)).astype(x.dtype)


def t5_bucket(dist):
    max_exact = N_BUCKETS // 2
    d = jnp.maximum(dist, 1).astype(jnp.float32)
    large = max_exact + (jnp.log(d / max_exact) / math.log(MAX_DISTANCE / max_exact)
                         * (N_BUCKETS - max_exact)).astype(jnp.int32)
    large = jnp.minimum(large, N_BUCKETS - 1)
    return jnp.where(dist < max_exact, dist, large)


def sliding_window_sink_attention(q, k, v, sinks, bias_table):
    b, p, _ = q.shape
    nb = p // BLOCK
    grp = A_HEADS // A_KV_HEADS
    qb = q.reshape(b, nb, BLOCK, A_KV_HEADS, grp, A_HEAD_DIM)
    kb = k.reshape(b, nb, BLOCK, A_KV_HEADS, A_HEAD_DIM)
    vb = v.reshape(b, nb, BLOCK, A_KV_HEADS, A_HEAD_DIM)

    def with_prev(t):
        prev = jnp.concatenate([jnp.zeros_like(t[:, :1]), t[:, :-1]], axis=1)
        return jnp.concatenate([prev, t], axis=2)

    kw, vw = with_prev(kb), with_prev(vb)
    s = jnp.einsum('bnqhgd,bnkhd->bnhgqk', qb, kw).astype(jnp.float32) * (A_HEAD_DIM ** -0.5)
    blk = jnp.arange(nb)[:, None] * BLOCK
    qpos = blk + jnp.arange(BLOCK)[None]
    kpos = blk - BLOCK + jnp.arange(2 * BLOCK)[None]
    dist = qpos[:, :, None] - kpos[:, None, :]
    allowed = (dist >= 0) & (dist < WINDOW) & (kpos[:, None, :] >= PAD)
    bias = bias_table[t5_bucket(jnp.maximum(dist, 0))]
    bias = bias.reshape(nb, BLOCK, 2 * BLOCK, A_KV_HEADS, grp).transpose(0, 3, 4, 1, 2)
    s = jnp.where(allowed[None, :, None, None], s + bias.astype(jnp.float32)[None], NEG)
    sink = sinks.astype(jnp.float32).reshape(A_KV_HEADS, grp)[None, None, :, :, None]
    m = jnp.maximum(s.max(axis=-1), sink)
    e = jnp.exp(s - m[..., None])
    pr = e / (e.sum(axis=-1, keepdims=True) + jnp.exp(sink - m)[..., None])
    o = jnp.einsum('bnhgqk,bnkhd->bnqhgd', pr.astype(v.dtype), vw)
    return o.reshape(b, p, A_HEADS * A_HEAD_DIM)


def indexed_sparse_mla(q_abs, c_kv, iq, ik, iw, w_uv, bias_table):
    b, p, _, _ = q_abs.shape
    nb = p // BLOCK
    topk = min(TOPK_MAX, (p - BLOCK) // 4)
    kpos = jnp.arange(p)
    gather = jax.vmap(lambda rows, idx: rows[idx])
    scale = B_HEAD_DIM ** -0.5

    def one_block(i):
        start = i * BLOCK
        qa = lax.dynamic_slice_in_dim(q_abs, start, BLOCK, axis=1)
        qi = lax.dynamic_slice_in_dim(iq, start, BLOCK, axis=1)
        wi = lax.dynamic_slice_in_dim(iw, start, BLOCK, axis=1)
        qpos = start + jnp.arange(BLOCK)
        logits = jnp.einsum('bqhd,bsd->bqhs', qi, ik)
        score = jnp.einsum('bqhs,bqh->bqs', jax.nn.relu(logits).astype(jnp.float32),
                           wi.astype(jnp.float32))
        admissible = (kpos[None, :] <= qpos[:, None]) & (kpos[None, :] >= PAD)
        score = jnp.where(admissible[None], score, -jnp.inf)
        _, sel = lax.top_k(score, topk)
        valid = (sel <= qpos[None, :, None]) & (sel >= PAD)
        c_sel = gather(c_kv, sel)
        s = jnp.einsum('bqhc,bqkc->bqhk', qa, c_sel).astype(jnp.float32) * scale
        bias = bias_table[t5_bucket(jnp.maximum(qpos[None, :, None] - sel, 0))]
        s = jnp.where(valid[:, :, None, :], s + jnp.moveaxis(bias, -1, 2).astype(jnp.float32), NEG)
        pr = jax.nn.softmax(s, axis=-1)
        return jnp.einsum('bqhk,bqkc->bqhc', pr.astype(c_kv.dtype), c_sel)

    lat = lax.map(one_block, jnp.arange(nb))
    lat = lat.transpose(1, 0, 2, 3, 4).reshape(b, p, B_HEADS, B_KV_RANK)
    o = jnp.einsum('bphc,chd->bphd', lat, w_uv)
    return o.reshape(b, p, B_HEADS * B_HEAD_DIM)


def setup_inputs(seed: int = 0) -> dict:
    key = jax.random.key(seed)
    ks = jax.random.split(key, 24)
    f32 = jnp.float32

    def dense(k, shape, fan_in):
        return jax.random.normal(k, shape, f32) * (fan_in ** -0.5)

    def gain(k, shape):
        return 1.0 + 0.05 * jax.random.normal(k, shape, f32)

    return {
        "x": jax.random.normal(ks[0], (BATCH, SEQ, D_MODEL), f32),
        "meta_tokens": jax.random.normal(ks[1], (N_META, D_MODEL), f32),
        "attn_norm_g": gain(ks[2], (DEPTH, D_MODEL)),
        "w_in": dense(ks[3], (DEPTH, D_MODEL, D_IN), D_MODEL),
        "b_gates": 0.02 * jax.random.normal(ks[4], (DEPTH, 2 * D_MODEL), f32),
        "q_norm_g": gain(ks[5], (DEPTH, B_Q_RANK)),
        "kv_norm_g": gain(ks[6], (DEPTH, B_KV_RANK)),
        "w_uq": dense(ks[7], (DEPTH, B_Q_RANK, B_HEADS * B_HEAD_DIM), B_Q_RANK),
        "w_uk": dense(ks[8], (DEPTH, B_KV_RANK, B_HEADS, B_HEAD_DIM), B_KV_RANK),
        "w_uv": dense(ks[9], (DEPTH, B_KV_RANK, B_HEADS, B_HEAD_DIM), B_KV_RANK),
        "idx_k_ln_g": gain(ks[10], (DEPTH, IDX_DIM)),
        "idx_k_ln_b": 0.02 * jax.random.normal(ks[11], (DEPTH, IDX_DIM), f32),
        "sinks": 0.5 * jax.random.normal(ks[12], (DEPTH, A_HEADS), f32),
        "rel_bias": 0.2 * jax.random.normal(ks[13], (N_BUCKETS, TOTAL_HEADS), f32),
        "w_branch_a": dense(ks[14], (DEPTH, A_HEADS * A_HEAD_DIM, D_MODEL), A_HEADS * A_HEAD_DIM),
        "w_branch_b": dense(ks[15], (DEPTH, B_HEADS * B_HEAD_DIM, D_MODEL), B_HEADS * B_HEAD_DIM),
        "w_out": dense(ks[16], (DEPTH, D_MODEL, D_MODEL), D_MODEL),
        "ffn_norm_g": gain(ks[17], (DEPTH, D_MODEL)),
        "w_ffn_gate": dense(ks[18], (DEPTH, D_MODEL, D_FF), D_MODEL),
        "w_ffn_up": dense(ks[19], (DEPTH, D_MODEL, D_FF), D_MODEL),
        "w_ffn_down": dense(ks[20], (DEPTH, D_FF, D_MODEL), D_FF),
        "final_norm_g": gain(ks[21], (D_MODEL,)),
    }


def reference(x, meta_tokens, attn_norm_g, w_in, b_gates, q_norm_g, kv_norm_g, w_uq, w_uk, w_uv,
              idx_k_ln_g, idx_k_ln_b, sinks, rel_bias, w_branch_a, w_branch_b, w_out,
              ffn_norm_g, w_ffn_gate, w_ffn_up, w_ffn_down, final_norm_g):
    b, seq, d = x.shape
    meta = jnp.broadcast_to(meta_tokens.astype(x.dtype)[None], (b, N_META, d))
    h = jnp.concatenate([jnp.zeros((b, PAD, d), x.dtype), meta, x], axis=1)
    p = h.shape[1]
    bias_a = rel_bias[:, :A_HEADS]
    bias_b = rel_bias[:, A_HEADS:]
    for l in range(DEPTH):
        hn = rms_norm(h, attn_norm_g[l])
        proj = hn @ w_in[l]
        aq, ak, av, bq, bkv, iq, ik, iw, gates = jnp.split(proj, IN_SPLITS, axis=-1)
        gates = jax.nn.sigmoid((gates + b_gates[l]).astype(jnp.float32)).astype(h.dtype)
        gate_a, gate_b = jnp.split(gates, 2, axis=-1)
        o_a = sliding_window_sink_attention(aq, ak, av, sinks[l], bias_a)
        q = (rms_norm(bq, q_norm_g[l]) @ w_uq[l]).reshape(b, p, B_HEADS, B_HEAD_DIM)
        c_kv = rms_norm(bkv, kv_norm_g[l])
        q_abs = jnp.einsum('bphd,chd->bphc', q, w_uk[l])
        iq = iq.reshape(b, p, IDX_HEADS, IDX_DIM)
        ik = layer_norm(ik, idx_k_ln_g[l], idx_k_ln_b[l])
        iw = iw * ((IDX_HEADS * IDX_DIM) ** -0.5)
        o_b = indexed_sparse_mla(q_abs, c_kv, iq, ik, iw, w_uv[l], bias_b)
        mixed = gate_a * (o_a @ w_branch_a[l]) + gate_b * (o_b @ w_branch_b[l])
        h = h + mixed @ w_out[l]
        hn = rms_norm(h, ffn_norm_g[l])
        h = h + (jax.nn.silu(hn @ w_ffn_gate[l]) * (hn @ w_ffn_up[l])) @ w_ffn_down[l]
    y = rms_norm(h, final_norm_g)
    return y[:, PAD + N_META:]
```

```python
import numpy as np
from contextlib import ExitStack
import concourse.bass as bass
import concourse.mybir as mybir
from concourse.bass_utils import run_bass_kernel_spmd

F32 = mybir.dt.float32
BF16 = mybir.dt.bfloat16
ALU = mybir.AluOpType
AF = mybir.ActivationFunctionType
AX = mybir.AxisListType


class _Op:
    __slots__ = ("idx", "eng", "fn", "waits", "dma_sem", "flagged", "sigval", "phase")


class Prog:
    ENGS = ("pe", "act", "dve", "pool", "sp")

    def __init__(self, nc, es):
        self.nc, self.es = nc, es
        self.ops = []
        self.eng_ops = {e: [] for e in self.ENGS}
        self.last_w = {}
        self.readers = {}
        self.dma_count = {}
        self.tail_fn = None
        self.phase = "setup"

    def sb(self, name, shape, dtype):
        return self.es.enter_context(self.nc.sbuf_tensor("sb_" + name, list(shape), dtype))

    def ps(self, name, shape, dtype):
        return self.es.enter_context(self.nc.psum_tensor("ps_" + name, list(shape), dtype))

    def _add(self, eng, fn, r, w, dma_sem):
        o = _Op()
        o.idx = len(self.ops)
        o.eng, o.fn, o.dma_sem = eng, fn, dma_sem
        o.phase = self.phase
        o.flagged = False
        o.sigval = 0
        deps = {}
        for k in r:
            d = self.last_w.get(k)
            if d is not None:
                deps[d] = True
        for k in w:
            d = self.last_w.get(k)
            if d is not None:
                deps.setdefault(d, False)
            for d in self.readers.get(k, ()):
                deps.setdefault(d, False)
        waits = []
        for d, raw in deps.items():
            do = self.ops[d]
            if do.dma_sem is not None:
                waits.append(("dma", do.dma_sem, 16 * self.dma_count[do.dma_sem]))
            elif do.eng == eng:
                if eng == "pe":
                    continue
                do.flagged = True
                waits.append(("eng", d))
            else:
                do.flagged = True
                waits.append(("eng", d))
        o.waits = waits
        for k in r:
            self.readers.setdefault(k, []).append(o.idx)
        for k in w:
            self.last_w[k] = o.idx
            self.readers[k] = []
        if dma_sem is not None:
            self.dma_count[dma_sem] = self.dma_count.get(dma_sem, 0) + 1
        self.ops.append(o)
        self.eng_ops[eng].append(o)
        return o

    def op(self, eng, fn, r=(), w=()):
        return self._add(eng, fn, r, w, None)

    def dma(self, eng, out, in_, r=(), w=(), sem=None, **kw):
        def fn(e, out=out, in_=in_, kw=kw):
            return e.dma_start(out=out, in_=in_, **kw)
        return self._add(eng, fn, r, w, sem)

    def barrier(self, dma_sems=()):
        last = {e: (self.eng_ops[e][-1] if self.eng_ops[e] else None) for e in self.ENGS}
        for e in self.ENGS:
            o = _Op()
            o.idx = len(self.ops)
            o.eng, o.fn, o.dma_sem = e, None, None
            o.flagged = False
            o.sigval = 0
            o.waits = []
            for e2 in self.ENGS:
                lo = last[e2]
                if e2 == e or lo is None:
                    continue
                while lo is not None and lo.fn is None:
                    k = self.eng_ops[e2].index(lo)
                    lo = self.eng_ops[e2][k - 1] if k > 0 else None
                if lo is None:
                    continue
                if lo.dma_sem is None:
                    lo.flagged = True
                    o.waits.append(("eng", lo.idx))
            for sname, cnt in self.dma_count.items():
                o.waits.append(("dma", sname, 16 * cnt))
            self.ops.append(o)
            self.eng_ops[e].append(o)

    def finish(self, out_sems):
        if self.tail_fn is not None:
            self.tail_fn()
        o = _Op()
        o.idx = len(self.ops)
        o.eng, o.fn, o.dma_sem = "sp", None, None
        o.flagged = False
        o.sigval = 0
        o.waits = [("dma", s, 16 * self.dma_count[s]) for s in out_sems]
        self.ops.append(o)
        self.eng_ops["sp"].append(o)
        self.emit()

    def emit(self):
        nc = self.nc
        sem_eng = {e: self.es.enter_context(nc.semaphore("sem_" + e)) for e in self.ENGS}
        sem_dma = {s: self.es.enter_context(nc.semaphore("sd_" + s)) for s in self.dma_count}
        for e in self.ENGS:
            c = 0
            for o in self.eng_ops[e]:
                if o.flagged:
                    c += 1
                    o.sigval = c

        def run(ename, e):
            seen = {}
            for o in self.eng_ops[ename]:
                for wt in o.waits:
                    if wt[0] == "dma":
                        key, sem, val = wt[1], sem_dma[wt[1]], wt[2]
                    else:
                        do = self.ops[wt[1]]
                        key, sem, val = "E" + do.eng, sem_eng[do.eng], do.sigval
                    if seen.get(key, 0) >= val:
                        continue
                    seen[key] = val
                    e.wait_ge(sem, val)
                if o.fn is None:
                    continue
                inst = o.fn(e)
                if o.dma_sem is not None:
                    inst.then_inc(sem_dma[o.dma_sem], 16)
                elif o.flagged:
                    inst.then_inc(sem_eng[ename], 1)

        with nc.Block() as block:
            @block.tensor
            def _(e):
                run("pe", e)

            @block.scalar
            def _(e):
                run("act", e)

            @block.vector
            def _(e):
                run("dve", e)

            @block.gpsimd
            def _(e):
                run("pool", e)

            @block.sync
            def _(e):
                run("sp", e)


D = 1024
DIN = 3524
NRES = 1476
DFF = 2816
NFC = 22
NPAD = 112
EPS = 1e-6
HEADPOS = [0, 2, 1, 3, 4, 6, 5, 7]

C_GATT, C_GFFN, C_BG, C_SINK, C_BFAR = 0, 8, 16, 32, 40
C_GQ, C_GKV, C_LNG, C_LNB, C_GFIN, C_NEGM, C_MASKA = 48, 304, 432, 496, 560, 1584, 1840
C_CTAB = 1840 + 384
NCONST = C_CTAB + 16

PC_UNIT = 3072
FF_UNIT = 2048
WD_UNIT = NFC * 128
S_PC = 0
S_FF = S_PC + 8 * 128 * PC_UNIT
S_WD = S_FF + NFC * 128 * FF_UNIT
S_TOT = S_WD + 8 * 128 * WD_UNIT


def _t5_bucket_np(dist):
    d = np.maximum(dist, 1).astype(np.float32)
    large = 16 + (np.log(d / np.float32(16)) / np.float32(np.log(128 / 16)) * np.float32(16)).astype(np.int32)
    large = np.minimum(large, 31)
    return np.where(dist < 16, dist, large)


class _Stop(Exception):
    pass


def build_nc(nseq, nblk, topk, n_iter=10, stop=0):
    nc = bass.Bass("TRN2", target_bir_lowering=False)
    T = nblk * 128
    NKB = nblk + 1
    NK = NKB * 128

    def din(name, shape, dt=F32):
        return nc.dram_tensor(name, list(shape), dt, kind="ExternalInput").ap()

    x_d = din("x", [nseq, T, D])
    meta_d = din("meta", [16, D])
    win_d = din("w_in", [D, DIN])
    wuq_d = din("w_uq", [256, 512])
    wuk_d = din("w_uk", [128, 512])
    wuv_d = din("w_uv", [128, 512])
    wa_d = din("w_a", [512, D])
    wb_d = din("w_b", [512, D])
    wout_d = din("w_out", [D, D])
    wg_d = din("wg", [D, DFF])
    wu_d = din("wu", [D, DFF])
    wd_d = din("wd", [DFF, D])
    cst_d = din("cst", [128, NCONST])
    bias_d = din("biasT", [128, 4 * 8 * 128])
    id_d = din("ident", [128, 128])
    y_d = nc.dram_tensor("y", [nseq, T, D], F32, kind="ExternalOutput").ap()
    scr = nc.dram_tensor("wscr", [S_TOT], BF16, kind="ExternalOutput").ap()

    def scr_view(off, n_units, unit):
        return scr[off:off + n_units * 128 * unit].rearrange("(u p e) -> u p e", p=128, e=unit)

    pc_s = scr_view(S_PC, 8, PC_UNIT)
    ff_s = scr_view(S_FF, NFC, FF_UNIT)
    wd_s = scr_view(S_WD, 8, WD_UNIT)

    with ExitStack() as es:
        P = Prog(nc, es)
        sb, op, dma = P.sb, P.op, P.dma

        w_in_sb = sb("w_in_sb", [128, 8, NRES], BF16)
        w_out_sb = sb("w_out_sb", [128, 8, D], BF16)
        w_uq_sb = sb("w_uq_sb", [128, 2, 512], BF16)
        w_ukTp = sb("w_ukTp", [128, 8, 128], BF16)
        w_uvp = sb("w_uvp", [128, 8, 128], BF16)
        cst = sb("cst", [128, NCONST], F32)
        ident = sb("ident_sb", [128, 128], BF16)
        EA = sb("EA", [128, 3, 8, 128], BF16)
        EB = sb("EB", [128, 2, 8, 128], BF16)
        hbg = sb("hbg", [128, 16], F32)
        nbfar = sb("nbfar", [128, 8], F32)
        esink = sb("esink", [128, 8], F32)
        ckvT = sb("ckvT", [128, NK], BF16)
        ckv1 = sb("ckv1", [128, NKB, 130], BF16)
        ikT = sb("ikT", [128, NK], BF16)
        akTp = sb("akTp", [128, 6, 4, 128], BF16)
        av1 = sb("av1", [128, 6, 2, 66], BF16)
        xh = sb("xh", [128, 4, D], F32)
        hnT = sb("hnT", [128, 8, 512], BF16)
        oaT = sb("oaT", [128, 4, 512], BF16)
        obT = sb("obT", [128, 4, 512], BF16)
        mixedT = sb("mixedT", [128, 8, 512], BF16)
        xb = sb("xb", [128, D], BF16)
        tmpf = sb("tmpf", [128, 512], F32)
        stat = sb("stat", [128, 32], F32)
        stat2 = sb("stat2", [128, 16], F32)
        bis = sb("bis", [128, 8], F32)
        iw_s = sb("iw_s", [128, 4, 4], F32)
        stream = sb("stream", [128, 2, PC_UNIT], BF16)
        akpad = sb("akpad", [128, 4, 128], BF16)
        iqpad = sb("iqpad", [128, 4, 128], BF16)
        ikdup = sb("ikdup", [128, 128], BF16)
        aq_tok = sb("aq_tok", [128, 512], BF16)
        qn_tok = sb("qn_tok", [128, 256], BF16)
        qnT = sb("qnT", [128, 2, 128], BF16)
        qT = sb("qT", [128, 4, 128], BF16)
        junk = sb("junk", [128, D], BF16)
        dtab = sb("dtab", [128, 16], F32)
        dtab2n = sb("dtab2n", [128, 16], F32)

        ARENA_F32 = (nc.sbuf_bytes_remaining - 256) // 4
        arena = sb("arena", [128, ARENA_F32], F32)

        class Carve:
            def __init__(self):
                self.off = 0

            def f32(self, n):
                a = arena[:, self.off:self.off + n]
                self.off += n
                assert self.off <= ARENA_F32, self.off
                return a

            def bf(self, n):
                assert n % 2 == 0
                return self.f32(n // 2).bitcast(BF16)

        cv = Carve()
        aqT = cv.bf(4 * 4 * 128).rearrange("p (j c q) -> p j c q", j=4, c=4)
        iqTp = cv.bf(4 * 4 * 128).rearrange("p (j c q) -> p j c q", j=4, c=4)
        qabsT = cv.bf(4 * 8 * 128).rearrange("p (j h q) -> p j h q", j=4, h=8)
        scores = cv.f32(NK)
        mask = cv.bf(NK)
        maskT = [cv.bf(NK).rearrange("p (k q) -> p k q", q=128) for _ in range(2)]
        eB = [cv.bf(1024).rearrange("p (h q) -> p h q", h=8) for _ in range(2)]
        pB = [cv.bf(1024).rearrange("p (h q) -> p h q", h=8) for _ in range(2)]
        lat = cv.bf(1024).rearrange("p (h q) -> p h q", h=8)
        latT = cv.bf(1024).rearrange("p (h q) -> p h q", h=8)
        oa_tok = cv.bf(512)
        rbuf = [cv.f32(512) for _ in range(2)]
        off_ab = cv.off
        cv.off = 0
        tAB = [cv.bf(1024).rearrange("p (a q) -> p a q", a=2) for _ in range(2)]
        mAB = [cv.f32(1024).rearrange("p (a q) -> p a q", a=2) for _ in range(2)]
        hn2T = cv.bf(8 * 512).rearrange("p (k q) -> p k q", k=8)
        actT = cv.bf(NFC * 512).rearrange("p (k q) -> p k q", k=NFC)
        wdb = [cv.bf(WD_UNIT).rearrange("p (k q) -> p k q", k=NFC) for _ in range(2)]
        tg = [tAB[i][:, 0, :] for i in range(2)]
        a1 = [mAB[i][:, 0, :] for i in range(2)]
        stg_bf = arena[:, :].bitcast(BF16)

        pend = []
        _sk = [0]

        def skey():
            _sk[0] += 1
            return "scrw%d" % _sk[0]

        def ck(n):
            for _ in range(2):
                if pend:
                    pend.pop(0)()
            if stop == n:
                raise _Stop()

        def _tail():
            P.op("dve", lambda e: e.memset(stat2[:, 15:16], 0.0), r=["stat2"], w=["stat2"])
        P.tail_fn = _tail

        pbk = [P.ps("pb%d" % i, [128, 512], F32) for i in range(8)]
        pT = pbk[7][:, :].bitcast(BF16)
        BK = ["B%d" % i for i in range(8)]

        dma("sp", cst[:, :], cst_d, w=["cst"], sem="s_cst")
        dma("pool", ident[:, :], id_d, w=["ident"], sem="s_id")
        for kc in range(8):
            dma("pool", w_in_sb[:, kc, :], win_d[kc * 128:(kc + 1) * 128, 0:NRES], w=["w_in_sb"], sem="s_win")
        dma("pool", w_out_sb[:, :, :], wout_d.rearrange("(k p) n -> p k n", p=128), w=["w_out_sb"], sem="s_wout")
        dma("pool", w_uq_sb[:, :, :], wuq_d.rearrange("(k p) n -> p k n", p=128), w=["w_uq_sb"], sem="s_wuq")
        op("dve", lambda e: e.memset(w_uvp[:, :, :], 0.0), w=["w_uvp"])
        wuv_v = wuv_d.rearrange("c (h d) -> c h d", d=64)
        for hh in range(2):
            for h in range(hh, 8, 2):
                dma("pool", w_uvp[:, h, hh * 64:(hh + 1) * 64], wuv_v[:, h, :], r=[], w=["w_uvp"], sem="s_wuv")
        wuk_tmp = stg_bf[:, 14336:14848]
        dma("pool", wuk_tmp, wuk_d, w=["stg0"], sem="s_stg0")
        op("dve", lambda e: e.memset(w_ukTp[:, :, :], 0.0), w=["w_ukTp"])
        for pr in range(4):
            op("pe", lambda e, pr=pr: e.transpose(pT[:, pr * 128:(pr + 1) * 128], wuk_tmp[:, pr * 128:(pr + 1) * 128], ident[:, :]),
               r=["stg0", "ident"], w=["B7"])
        for pr in range(4):
            op("dve", lambda e, pr=pr: e.tensor_copy(w_ukTp[0:64, 2 * pr, :], pT[0:64, pr * 128:(pr + 1) * 128]), r=["B7"], w=["w_ukTp"])
            op("dve", lambda e, pr=pr: e.tensor_copy(w_ukTp[64:128, 2 * pr + 1, :], pT[64:128, pr * 128:(pr + 1) * 128]), r=["B7"], w=["w_ukTp"])
        op("dve", lambda e: e.tensor_scalar(hbg[:, :], cst[:, C_BG:C_BG + 16], 0.5, None, ALU.mult), r=["cst"], w=["hbg"])
        op("dve", lambda e: e.tensor_scalar(nbfar[:, :], cst[:, C_BFAR:C_BFAR + 8], -1.0, None, ALU.mult), r=["cst"], w=["nbfar"])
        op("act", lambda e: e.activation(esink[:, :], cst[:, C_SINK:C_SINK + 8], AF.Exp), r=["cst"], w=["esink"])
        op("dve", lambda e: e.memset(stat[:, :], 1.0), w=["stat"])
        op("dve", lambda e: e.memset(stat2[:, :], 1.0), w=["stat2"])
        op("dve", lambda e: e.memset(bis[:, :], 0.0), w=["bis"])
        op("pool", lambda e: e.memset(akpad[:, :, :], 0.0), w=["akpad"])
        op("pool", lambda e: e.memset(iqpad[:, :, :], 0.0), w=["iqpad"])
        op("pool", lambda e: e.memset(av1[:, :, :, 64:65], 1.0), w=["av1"])
        op("pool", lambda e: e.memset(ckv1[:, :, 128:129], 1.0), w=["ckv1"])
        if stop == 1:
            P.finish([])
            return nc
        btmp = arena[:, 0:4096].rearrange("p (v h q) -> p v h q", v=4, h=8)
        etmp = arena[:, 4096:4096 + 3072].rearrange("p (v h q) -> p v h q", v=3, h=8)
        dma("sp", arena[:, 0:4096], bias_d, w=["btmp"], sem="s_btmp")
        op("act", lambda e: e.activation(etmp[:, 0, :, :], btmp[:, 0, :, :], AF.Exp), r=["btmp"], w=["etmp0"])
        op("act", lambda e: e.activation(etmp[:, 1, :, :], btmp[:, 1, :, :], AF.Exp), r=["btmp"], w=["etmp1"])
        mA_ = cst[:, C_MASKA:C_MASKA + 384].rearrange("p (v q) -> p v q", v=3)

        def mbc(v):
            return mA_[:, v:v + 1, :].broadcast_to([128, 8, 128])

        op("dve", lambda e: e.tensor_tensor(EA[:, 0, :, :], etmp[:, 0, :, :], mbc(0), ALU.mult), r=["etmp0", "cst"], w=["EA"])
        op("dve", lambda e: e.tensor_tensor(EA[:, 1, :, :], etmp[:, 1, :, :], mbc(1), ALU.mult), r=["etmp1", "cst"], w=["EA"])
        op("dve", lambda e: e.tensor_tensor(EA[:, 2, :, :], etmp[:, 1, :, :], mbc(2), ALU.mult), r=["etmp1", "cst"], w=["EA"])
        for v in range(2):
            for h in range(8):
                op("act", lambda e, v=v, h=h: e.activation(etmp[:, 2, h, :], btmp[:, 2 + v, h, :], AF.Exp, bias=nbfar[:, h:h + 1]),
                   r=["btmp", "nbfar", "EB"], w=["etmp2"])
            if v == 0:
                op("dve", lambda e: e.tensor_tensor(EB[:, 0, :, :], etmp[:, 2, :, :], mbc(0), ALU.mult), r=["etmp2", "cst"], w=["EB"])
            else:
                op("dve", lambda e: e.tensor_copy(EB[:, 1, :, :], etmp[:, 2, :, :]), r=["etmp2"], w=["EB"])
        P.barrier()
        if stop == 2:
            P.finish([])
            return nc

        g_stg = stg_bf[:, 0:16384].rearrange("p (k n) -> p k n", k=8)
        a_stg = stg_bf[:, 16384:20480].rearrange("p (k n) -> p k n", k=4)
        b_stg = stg_bf[:, 20480:24576].rearrange("p (k n) -> p k n", k=4)
        for kc in range(8):
            dma("pool", g_stg[:, kc, :], win_d[kc * 128:(kc + 1) * 128, NRES:DIN], w=["stgA"], sem="s_stgA")
        dma("pool", a_stg, wa_d.rearrange("(k p) n -> p k n", p=128), w=["stgA"], sem="s_stgA")
        dma("pool", b_stg, wb_d.rearrange("(k p) n -> p k n", p=128), w=["stgA"], sem="s_stgA")
        for c in range(8):
            u = pc_s[c]
            dma("sp", u[:, 0:512].rearrange("p (k f) -> p k f", k=4), a_stg[:, :, c * 128:(c + 1) * 128], r=["stgA"], w=[skey()], sem="s_scr")
            dma("sp", u[:, 512:1024].rearrange("p (k f) -> p k f", k=4), b_stg[:, :, c * 128:(c + 1) * 128], r=["stgA"], w=[skey()], sem="s_scr")
            dma("sp", u[:, 1024:2048].rearrange("p (k f) -> p k f", k=8), g_stg[:, :, c * 128:(c + 1) * 128], r=["stgA"], w=[skey()], sem="s_scr")
            dma("sp", u[:, 2048:3072].rearrange("p (k f) -> p k f", k=8), g_stg[:, :, 1024 + c * 128:1024 + (c + 1) * 128], r=["stgA"], w=[skey()], sem="s_scr")
        d_stg = stg_bf[:, 0:NFC * 1024].rearrange("p (k n) -> p k n", k=NFC)
        for k0 in range(0, NFC, 11):
            dma("pool", d_stg[:, k0:k0 + 11, :], wd_d[k0 * 128:(k0 + 11) * 128, :].rearrange("(k p) n -> p k n", p=128),
                r=[], w=["stgA"], sem="s_stgA")
        for q in range(8):
            dma("sp", wd_s[q].rearrange("p (k f) -> p k f", k=NFC), d_stg[:, :, q * 128:(q + 1) * 128], r=["stgA"], w=[skey()], sem="s_scr")
        for hf in range(2):
            g2 = stg_bf[:, 0:8 * 1408].rearrange("p (k n) -> p k n", k=8)
            u2 = stg_bf[:, 8 * 1408:16 * 1408].rearrange("p (k n) -> p k n", k=8)
            for kc in range(8):
                dma("pool", g2[:, kc, :], wg_d[kc * 128:(kc + 1) * 128, hf * 1408:(hf + 1) * 1408], r=[], w=["stgA"], sem="s_stgA")
                dma("pool", u2[:, kc, :], wu_d[kc * 128:(kc + 1) * 128, hf * 1408:(hf + 1) * 1408], r=[], w=["stgA"], sem="s_stgA")
            for i in range(11):
                u = ff_s[hf * 11 + i]
                dma("sp", u[:, 0:1024].rearrange("p (k f) -> p k f", k=8), g2[:, :, i * 128:(i + 1) * 128], r=["stgA"], w=[skey()], sem="s_scr")
                dma("sp", u[:, 1024:2048].rearrange("p (k f) -> p k f", k=8), u2[:, :, i * 128:(i + 1) * 128], r=["stgA"], w=[skey()], sem="s_scr")
        P.barrier(dma_sems=["s_scr", "s_stgA"])
        if stop == 3:
            P.finish([])
            return nc

        def rstd_from(out_ap, in_ap, scale, keys):
            op("dve", lambda e: e.tensor_scalar(out_ap, in_ap, scale, EPS, ALU.mult, ALU.add), r=keys, w=keys)
            op("act", lambda e: e.activation(out_ap, out_ap, AF.Sqrt), r=keys, w=keys)
            op("dve", lambda e: e.reciprocal(out_ap, out_ap), r=keys, w=keys)

        def norm_T(src_ap, rstd_ap, gcol, dstT, skeys, dkeys):
            op("dve", lambda e: e.tensor_scalar(xb[:, :], src_ap, rstd_ap, None, ALU.mult), r=skeys, w=["xb"])
            for kc in range(8):
                op("pe", lambda e, kc=kc: e.transpose(pT[:, kc * 128:(kc + 1) * 128], xb[:, kc * 128:(kc + 1) * 128], ident[:, :]),
                   r=["xb", "ident"], w=["B7"])
            gb = cst[:, gcol:gcol + 8].unsqueeze(2).broadcast_to([128, 8, 128])
            op("dve", lambda e: e.tensor_tensor(dstT, pT[:, :].rearrange("p (k q) -> p k q", k=8), gb, ALU.mult),
               r=["B7", "cst"], w=dkeys)

        def tile_A(s, t):
            j = (t - 1) % 4 if t >= 1 else 0
            slot = t % 6
            xk = "xh%d" % j
            if t == 0:
                op("pool", lambda e: e.memset(xh[:, 0, :], 0.0), w=[xk])
                dma("sp", xh[NPAD:128, 0, :], meta_d, w=[xk], sem="s_x0")
            else:
                dma("sp", xh[:, j, :], x_d[s, (t - 1) * 128:t * 128, :], w=[xk], sem="s_x%d" % j)
            op("act", lambda e: e.activation(junk[:, :], xh[:, j, :], AF.Square, accum_out=stat[:, 0:1]), r=[xk], w=["junk", "stat"])
            rstd_from(stat[:, 0:1], stat[:, 0:1], 1.0 / D, ["stat"])
            ck(41)
            hT = hnT[:, :, j * 128:(j + 1) * 128]
            norm_T(xh[:, j, :], stat[:, 0:1], C_GATT, hT, [xk, "stat"], ["hnT%d" % j])
            ck(42)
            cols = [(0, 512), (512, 1024), (1024, NRES)]
            if t == 0:
                cols = cols[1:]
            for bi, (c0, c1) in enumerate(cols):
                bk = bi if t >= 1 else bi + 1
                for kc in range(8):
                    op("pe", lambda e, kc=kc, c0=c0, c1=c1, bk=bk: e.matmul(pbk[bk][:, 0:c1 - c0], lhsT=hnT[:, kc, j * 128:(j + 1) * 128],
                                                                          rhs=w_in_sb[:, kc, c0:c1], start=(kc == 0), stop=(kc == 7)),
                       r=["hnT%d" % j, "w_in_sb"], w=[BK[bk]])
            ck(43)
            for g in range(2):
                src = pbk[1][:, g * 64:(g + 1) * 64]
                op("act", lambda e, g=g, src=src: e.activation(akpad[:, 2 * g, 0:64], src, AF.Copy), r=["B1"], w=["akpad"])
                op("act", lambda e, g=g, src=src: e.activation(akpad[:, 2 * g + 1, 64:128], src, AF.Copy), r=["B1"], w=["akpad"])
            ck(4311)
            for g in range(2):
                op("act", lambda e, g=g: e.activation(av1[:, slot, g, 0:64], pbk[1][:, 128 + g * 64:128 + (g + 1) * 64], AF.Copy), r=["B1"], w=["av1"])
            ck(431)
            op("act", lambda e: e.activation(tmpf[:, 256:384], pbk[2][:, 0:128], AF.Square, accum_out=stat[:, 2:3]), r=["B2"], w=["tmpf_kv", "stat"])
            op("dve", lambda e: e.tensor_scalar(junk[:, 0:64], pbk[2][:, 384:448], 1.0 / 64, None, ALU.mult, ALU.add, accum_out=stat[:, 3:4]),
               r=["B2"], w=["junk", "stat"])
            op("dve", lambda e: e.tensor_scalar(tmpf[:, 384:448], pbk[2][:, 384:448], stat[:, 3:4], None, ALU.subtract), r=["B2", "stat"], w=["tmpf_ik"])
            op("act", lambda e: e.activation(junk[:, 0:64], tmpf[:, 384:448], AF.Square, accum_out=stat[:, 4:5]), r=["tmpf_ik"], w=["junk", "stat"])
            if t >= 1:
                op("act", lambda e: e.activation(junk[:, 0:256], pbk[1][:, 256:512], AF.Square, accum_out=stat[:, 1:2]), r=["B1"], w=["junk", "stat"])
            ck(432)
            op("dve", lambda e: e.tensor_scalar(stat[:, 1:2], stat[:, 1:2], 0.5, None, ALU.mult), r=["stat"], w=["stat"])
            op("dve", lambda e: e.tensor_scalar(stat[:, 4:5], stat[:, 4:5], 2.0, None, ALU.mult), r=["stat"], w=["stat"])
            op("dve", lambda e: e.tensor_copy(stat[:, 8:9], stat[:, 1:2]), r=["stat"], w=["stat"])
            op("dve", lambda e: e.tensor_copy(stat[:, 9:10], stat[:, 2:3]), r=["stat"], w=["stat"])
            op("dve", lambda e: e.tensor_copy(stat[:, 10:11], stat[:, 4:5]), r=["stat"], w=["stat"])
            rstd_from(stat[:, 8:11], stat[:, 8:11], 1.0 / 128, ["stat"])
            ck(433)
            op("dve", lambda e: e.scalar_tensor_tensor(ckv1[:, t, 0:128], pbk[2][:, 0:128], stat[:, 9:10], cst[:, C_GKV:C_GKV + 128], ALU.mult, ALU.mult),
               r=["B2", "stat", "cst"], w=["ckv1"])
            op("dve", lambda e: e.scalar_tensor_tensor(tmpf[:, 448:512], tmpf[:, 384:448], stat[:, 10:11], cst[:, C_LNG:C_LNG + 64], ALU.mult, ALU.mult),
               r=["tmpf_ik", "stat", "cst"], w=["junk2"])
            op("dve", lambda e: e.tensor_tensor(ikdup[:, 0:64], tmpf[:, 448:512], cst[:, C_LNB:C_LNB + 64], ALU.add), r=["junk2", "cst"], w=["ikdup"])
            op("dve", lambda e: e.tensor_copy(ikdup[:, 64:128], ikdup[:, 0:64]), r=["ikdup"], w=["ikdup"])
            ck(44)
            for v in range(4):
                op("pe", lambda e, v=v: e.transpose(pT[:, v * 128:(v + 1) * 128], akpad[:, v, :], ident[:, :]), r=["akpad", "ident"], w=["B7"])
            op("pe", lambda e: e.transpose(pT[:, 512:640], ckv1[:, t, 0:128], ident[:, :]), r=["ckv1", "ident"], w=["B7"])
            op("pe", lambda e: e.transpose(pT[:, 640:768], ikdup[:, :], ident[:, :]), r=["ikdup", "ident"], w=["B7"])
            ck(45)
            op("act", lambda e: e.activation(akTp[:, slot, :, :], pT[:, 0:512].rearrange("p (v q) -> p v q", v=4), AF.Copy), r=["B7"], w=["akTp"])
            ck(46)
            op("act", lambda e: e.activation(ckvT[:, t * 128:(t + 1) * 128], pT[:, 512:640], AF.Copy), r=["B7"], w=["ckvT"])
            op("act", lambda e: e.activation(ikT[:, t * 128:(t + 1) * 128], pT[:, 640:768], AF.Copy), r=["B7"], w=["ikT"])
            if t == 0:
                return
            op("act", lambda e: e.activation(aq_tok[:, :], pbk[0][:, :], AF.Copy), r=["B0"], w=["aq_tok"])
            op("dve", lambda e: e.scalar_tensor_tensor(qn_tok[:, :], pbk[1][:, 256:512], stat[:, 8:9], cst[:, C_GQ:C_GQ + 256], ALU.mult, ALU.mult),
               r=["B1", "stat", "cst"], w=["qn_tok"])
            for h in range(4):
                hh = h % 2
                op("act", lambda e, h=h, hh=hh: e.activation(iqpad[:, h, hh * 64:(hh + 1) * 64], pbk[2][:, 128 + h * 64:128 + (h + 1) * 64], AF.Copy),
                   r=["B2"], w=["iqpad"])
            op("act", lambda e: e.activation(iw_s[:, j, :], pbk[2][:, 448:452], AF.Copy, scale=1.0 / 16), r=["B2"], w=["iw_s"])
            for c in range(4):
                op("pe", lambda e, c=c: e.transpose(pT[:, c * 128:(c + 1) * 128], aq_tok[:, c * 128:(c + 1) * 128], ident[:, :]), r=["aq_tok", "ident"], w=["B7"])
            for c in range(2):
                op("pe", lambda e, c=c: e.transpose(pT[:, 512 + c * 128:512 + (c + 1) * 128], qn_tok[:, c * 128:(c + 1) * 128], ident[:, :]),
                   r=["qn_tok", "ident"], w=["B7"])
            op("act", lambda e: e.activation(aqT[:, j, :, :], pT[:, 0:512].rearrange("p (c q) -> p c q", c=4), AF.Copy), r=["B7"], w=["aqT%d" % j])
            op("act", lambda e: e.activation(qnT[:, :, :], pT[:, 512:768].rearrange("p (c q) -> p c q", c=2), AF.Copy), r=["B7"], w=["qnT"])
            for v in range(4):
                op("pe", lambda e, v=v: e.transpose(pT[:, v * 128:(v + 1) * 128], iqpad[:, v, :], ident[:, :]), r=["iqpad", "ident"], w=["B7"])
            op("act", lambda e: e.activation(iqTp[:, j, :, :], pT[:, 0:512].rearrange("p (v q) -> p v q", v=4), AF.Copy), r=["B7"], w=["iqTp%d" % j])
            for f in range(4):
                for rc in range(2):
                    op("pe", lambda e, f=f, rc=rc: e.matmul(pbk[3][:, f * 128:(f + 1) * 128], lhsT=w_uq_sb[:, rc, f * 128:(f + 1) * 128], rhs=qnT[:, rc, :],
                                                             start=(rc == 0), stop=(rc == 1)), r=["w_uq_sb", "qnT"], w=["B3"])
            op("act", lambda e: e.activation(qT[:, :, :], pbk[3][:, :].rearrange("p (c q) -> p c q", c=4), AF.Copy), r=["B3"], w=["qT"])
            for h in range(8):
                bk = 4 + h // 4
                op("pe", lambda e, h=h, bk=bk: e.matmul(pbk[bk][:, (h % 4) * 128:(h % 4 + 1) * 128], lhsT=w_ukTp[:, h, :], rhs=qT[:, h // 2, :],
                                                         start=True, stop=True), r=["w_ukTp", "qT"], w=[BK[bk]])
            op("act", lambda e: e.activation(qabsT[:, j, 0:4, :], pbk[4][:, :].rearrange("p (h q) -> p h q", h=4), AF.Copy), r=["B4"], w=["qabsT%d" % j])
            op("act", lambda e: e.activation(qabsT[:, j, 4:8, :], pbk[5][:, :].rearrange("p (h q) -> p h q", h=4), AF.Copy), r=["B5"], w=["qabsT%d" % j])

        def tile_B(s, t):
            j = (t - 1) % 4
            nk = t + 1
            N = nk * 128
            kbs = [t - 1, t]
            for ki, kb in enumerate(kbs):
                slot = kb % 6
                b0 = 2 * ki
                for g in range(2):
                    for half in range(2):
                        op("pe", lambda e, g=g, half=half, slot=slot, b0=b0: e.matmul(
                            pbk[b0 + g][:, half * 256:(half + 1) * 256], lhsT=akTp[:, slot, 2 * g + half, :],
                            rhs=aqT[:, j, 2 * g:2 * g + 2, :], start=True, stop=True),
                           r=["akTp", "aqT%d" % j], w=[BK[b0 + g]])
                for g in range(2):
                    op("act", lambda e, g=g, ki=ki, b0=b0: e.activation(eB[ki][:, 4 * g:4 * g + 4, :], pbk[b0 + g][:, :].rearrange("p (h q) -> p h q", h=4),
                                                                        AF.Exp, scale=0.125), r=[BK[b0 + g]], w=["eB%d" % ki])
                var = 0 if kb == t else (2 if kb == 0 else 1)
                op("dve", lambda e, ki=ki, var=var: e.tensor_tensor(pB[ki][:, :, :], eB[ki][:, :, :], EA[:, var, :, :], ALU.mult),
                   r=["eB%d" % ki, "EA"], w=["pB%d" % ki])
            for ki, kb in enumerate(kbs):
                slot = kb % 6
                for pos in range(8):
                    g = pos // 4
                    op("pe", lambda e, pos=pos, g=g, ki=ki, slot=slot: e.matmul(
                        pbk[4 + g][:, (pos % 4) * 65:(pos % 4) * 65 + 65], lhsT=pB[ki][:, pos, :], rhs=av1[:, slot, g, 0:65],
                        start=(ki == 0 and pos % 4 == 0), stop=(ki == 1 and pos % 4 == 3), skip_group_check=True), r=["pB%d" % ki, "av1"], w=[BK[4 + g]])
            for g in range(2):
                pv = pbk[4 + g][:, 0:260].rearrange("p (h d) -> p h d", h=4)
                op("dve", lambda e, g=g, pv=pv: e.tensor_tensor(stat2[:, 4 * g:4 * g + 4], pv[:, :, 64:65].rearrange("p h o -> p (h o)"),
                                                                esink[:, 4 * g:4 * g + 4], ALU.add), r=[BK[4 + g], "esink"], w=["stat2"])
            op("dve", lambda e: e.reciprocal(stat2[:, 0:8], stat2[:, 0:8]), r=["stat2"], w=["stat2"])
            for g in range(2):
                pv = pbk[4 + g][:, 0:260].rearrange("p (h d) -> p h d", h=4)
                op("dve", lambda e, g=g, pv=pv: e.tensor_tensor(oa_tok[:, g * 256:(g + 1) * 256].rearrange("p (h d) -> p h d", h=4), pv[:, :, 0:64],
                                                                stat2[:, 4 * g:4 * g + 4].unsqueeze(2).broadcast_to([128, 4, 64]), ALU.mult),
                   r=[BK[4 + g], "stat2"], w=["oa_tok"])
            for c in range(4):
                op("pe", lambda e, c=c: e.transpose(pT[:, c * 128:(c + 1) * 128], oa_tok[:, c * 128:(c + 1) * 128], ident[:, :]), r=["oa_tok", "ident"], w=["B7"])
            op("act", lambda e: e.activation(oaT[:, :, j * 128:(j + 1) * 128], pT[:, 0:512].rearrange("p (c q) -> p c q", c=4), AF.Copy),
               r=["B7"], w=["oaT%d" % j])
            grp = 0
            for k0 in range(0, N, 512):
                k1 = min(N, k0 + 512)
                for h in range(4):
                    bk = grp % 4
                    rb = grp % 2
                    grp += 1
                    op("pe", lambda e, h=h, bk=bk, k0=k0, k1=k1: e.matmul(pbk[bk][:, 0:k1 - k0], lhsT=iqTp[:, j, h, :], rhs=ikT[:, k0:k1], start=True, stop=True),
                       r=["iqTp%d" % j, "ikT"], w=[BK[bk]])
                    op("act", lambda e, bk=bk, rb=rb, k0=k0, k1=k1: e.activation(rbuf[rb][:, 0:k1 - k0], pbk[bk][:, 0:k1 - k0], AF.Relu), r=[BK[bk]], w=["rbuf%d" % rb])
                    if h == 0:
                        op("dve", lambda e, rb=rb, k0=k0, k1=k1: e.tensor_scalar(scores[:, k0:k1], rbuf[rb][:, 0:k1 - k0], iw_s[:, j, 0:1], None, ALU.mult),
                           r=["rbuf%d" % rb, "iw_s"], w=["scores"])
                    else:
                        op("dve", lambda e, rb=rb, k0=k0, k1=k1, h=h: e.scalar_tensor_tensor(scores[:, k0:k1], rbuf[rb][:, 0:k1 - k0], iw_s[:, j, h:h + 1],
                                                                                             scores[:, k0:k1], ALU.mult, ALU.add),
                           r=["rbuf%d" % rb, "iw_s", "scores"], w=["scores"])
            op("dve", lambda e: e.tensor_reduce(bis[:, 0:1], scores[:, 0:N], AX.X, ALU.max, apply_absolute_value=True), r=["scores"], w=["bisR"])
            negm = cst[:, C_NEGM:C_NEGM + 256].rearrange("p (v k) -> p v k", v=2)
            op("dve", lambda e: e.tensor_tensor(scores[:, t * 128:(t + 1) * 128], scores[:, t * 128:(t + 1) * 128], negm[:, 0, :], ALU.add), r=["scores", "cst"], w=["scores"])
            op("dve", lambda e: e.tensor_tensor(scores[:, 0:128], scores[:, 0:128], negm[:, 1, :], ALU.add), r=["scores", "cst"], w=["scores"])
            if t * 128 + 16 > topk:
                op("dve", lambda e: e.tensor_scalar(dtab[:, 0:n_iter], cst[:, C_CTAB:C_CTAB + n_iter], bis[:, 0:1], None, ALU.mult), r=["bisR", "cst"], w=["dtab"])
                op("dve", lambda e: e.tensor_scalar(dtab2n[:, 0:n_iter], dtab[:, 0:n_iter], -2.0, None, ALU.mult), r=["dtab"], w=["dtab2n"])
                op("dve", lambda e: e.memset(bis[:, 2:3], 0.0), r=[], w=["bisnm"])

        def bis_steps(t):
            N = (t + 1) * 128
            steps = []
            if t * 128 + 16 <= topk:
                return steps
            thr_s = float(2 * topk - N) - 0.5
            for it in range(n_iter):
                def step(it=it):
                    if it % 2 == 0:
                        op("act", lambda e: e.activation(mask[:, 0:N], scores[:, 0:N], AF.Sign, bias=bis[:, 2:3], accum_out=bis[:, 3:4]),
                           r=["scores", "bisnm"], w=["mask", "biscnt"])
                        op("dve", lambda e: e.scalar_tensor_tensor(bis[:, 4:5], bis[:, 3:4], thr_s, dtab2n[:, it:it + 1], ALU.is_ge, ALU.mult),
                           r=["biscnt", "dtab2n"], w=["bisu"])
                    else:
                        op("dve", lambda e: e.tensor_scalar(bis[:, 5:6], bis[:, 2:3], -1.0, None, ALU.mult), r=["bisnm"], w=["bismid"])
                        op("dve", lambda e: e.tensor_scalar(mask[:, 0:N], scores[:, 0:N], bis[:, 5:6], None, ALU.is_ge, ALU.add, accum_out=bis[:, 3:4]),
                           r=["scores", "bismid"], w=["mask", "biscnt"])
                        op("dve", lambda e: e.scalar_tensor_tensor(bis[:, 4:5], bis[:, 3:4], float(topk) - 0.5, dtab2n[:, it:it + 1], ALU.is_ge, ALU.mult),
                           r=["biscnt", "dtab2n"], w=["bisu"])
                    op("dve", lambda e: e.scalar_tensor_tensor(bis[:, 2:3], bis[:, 4:5], bis[:, 2:3], dtab[:, it:it + 1], ALU.add, ALU.add),
                       r=["bisu", "bisnm", "dtab"], w=["bisnm"])
                steps.append(step)
            return steps

        def tile_B1post(s, t):
            j = (t - 1) % 4
            par = j % 2
            nk = t + 1
            N = nk * 128
            if t * 128 + 16 <= topk:
                op("dve", lambda e: e.memset(bis[:, 1:2], -1e29), r=[], w=["bisthr"])
            else:
                op("dve", lambda e: e.scalar_tensor_tensor(bis[:, 1:2], bis[:, 2:3], -1.0, dtab[:, n_iter - 1:n_iter], ALU.mult, ALU.subtract),
                   r=["bisnm", "dtab"], w=["bisthr"])
            op("dve", lambda e: e.tensor_scalar(mask[:, 0:N], scores[:, 0:N], bis[:, 1:2], None, ALU.is_ge), r=["scores", "bisthr"], w=["mask"])
            for k0 in range(0, nk, 8):
                k1 = min(nk, k0 + 8)
                for kc in range(k0, k1):
                    op("pe", lambda e, kc=kc, k0=k0: e.transpose(pT[:, (kc - k0) * 128:(kc - k0 + 1) * 128], mask[:, kc * 128:(kc + 1) * 128], ident[:, :]),
                       r=["mask", "ident"], w=["B7"])
                op("act", lambda e, k0=k0, k1=k1: e.activation(maskT[par][:, k0:k1, :], pT[:, 0:(k1 - k0) * 128].rearrange("p (k q) -> p k q", q=128), AF.Copy),
                   r=["B7"], w=["maskT%d" % par])

        def tile_B2(s, t, steps):
            j = (t - 1) % 4
            par = j % 2
            nk = t + 1
            for kc in range(nk):
                pp = kc % 2
                b0 = 2 * pp
                for hh in range(2):
                    op("pe", lambda e, kc=kc, hh=hh, b0=b0: e.matmul(pbk[b0 + hh][:, :], lhsT=ckvT[:, kc * 128:(kc + 1) * 128],
                                                                     rhs=qabsT[:, j, 4 * hh:4 * hh + 4, :], start=True, stop=True),
                       r=["ckvT", "qabsT%d" % j], w=[BK[b0 + hh]])
                    op("act", lambda e, hh=hh, pp=pp, b0=b0: e.activation(eB[pp][:, 4 * hh:4 * hh + 4, :], pbk[b0 + hh][:, :].rearrange("p (h q) -> p h q", h=4),
                                                                           AF.Exp, scale=0.125), r=[BK[b0 + hh]], w=["eB%d" % pp])
                if kc >= t - 1:
                    vi = 0 if kc == t else 1
                    op("dve", lambda e, pp=pp, vi=vi: e.tensor_tensor(eB[pp][:, :, :], eB[pp][:, :, :], EB[:, vi, :, :], ALU.mult),
                       r=["eB%d" % pp, "EB"], w=["eB%d" % pp])
                op("dve", lambda e, pp=pp, kc=kc: e.tensor_tensor(pB[pp][:, :, :], eB[pp][:, :, :], maskT[par][:, kc:kc + 1, :].broadcast_to([128, 8, 128]), ALU.mult),
                   r=["eB%d" % pp, "maskT%d" % par], w=["pB%d" % pp])
                for h in range(8):
                    bk = 4 + h // 3
                    o0 = (h % 3) * 129
                    op("pe", lambda e, h=h, bk=bk, o0=o0, pp=pp, kc=kc: e.matmul(pbk[bk][:, o0:o0 + 129], lhsT=pB[pp][:, h, :], rhs=ckv1[:, kc, 0:129],
                                                                                 start=(kc == 0 and h % 3 == 0), stop=(kc == nk - 1 and (h % 3 == 2 or h == 7)),
                                                                                 skip_group_check=True),
                       r=["pB%d" % pp, "ckv1"], w=[BK[bk]])
                if steps:
                    steps.pop(0)()
            while steps:
                steps.pop(0)()
            for b in range(3):
                nh = 3 if b < 2 else 2
                pv = pbk[4 + b][:, 0:nh * 129].rearrange("p (h d) -> p h d", h=nh)
                op("act", lambda e, b=b, nh=nh, pv=pv: e.activation(stat2[:, 8 + 3 * b:8 + 3 * b + nh], pv[:, :, 128:129].rearrange("p h o -> p (h o)"), AF.Copy),
                   r=[BK[4 + b]], w=["stat2"])
                op("dve", lambda e, b=b, nh=nh: e.reciprocal(stat2[:, 8 + 3 * b:8 + 3 * b + nh], stat2[:, 8 + 3 * b:8 + 3 * b + nh]), r=["stat2"], w=["stat2"])
                op("dve", lambda e, b=b, nh=nh, pv=pv: e.tensor_tensor(lat[:, 3 * b:3 * b + nh, :], pv[:, :, 0:128],
                                                                       stat2[:, 8 + 3 * b:8 + 3 * b + nh].unsqueeze(2).broadcast_to([128, nh, 128]), ALU.mult),
                   r=[BK[4 + b], "stat2"], w=["lat"])
            for h in range(8):
                op("pe", lambda e, h=h: e.transpose(pT[:, h * 128:(h + 1) * 128], lat[:, h, :], ident[:, :]), r=["lat", "ident"], w=["B7"])
            op("act", lambda e: e.activation(latT[:, :, :], pT[:, :].rearrange("p (h q) -> p h q", h=8), AF.Copy), r=["B7"], w=["latT"])
            for h in range(8):
                op("pe", lambda e, h=h: e.matmul(pbk[0][:, (h // 2) * 128:(h // 2 + 1) * 128], lhsT=w_uvp[:, h, :], rhs=latT[:, h, :],
                                                  start=(h % 2 == 0), stop=(h % 2 == 1)), r=["w_uvp", "latT"], w=["B0"])
            op("act", lambda e: e.activation(obT[:, :, j * 128:(j + 1) * 128], pbk[0][:, :].rearrange("p (c q) -> p c q", c=4), AF.Copy),
               r=["B0"], w=["obT%d" % j])

        def phase_C(s, b):
            for c in range(8):
                sl = c % 2
                skc = ["sq0", "sq1", "sq2"] if sl == 0 else ["sq3", "sq4", "sq5"]
                dma("sp", stream[:, sl, :], pc_s[c], r=["scr"], w=skc, sem="s_st%d" % sl)
                U = stream[:, sl, :]
                b0 = 4 * sl if sl == 0 else 3
                banks = [0, 1, 2, 3] if sl == 0 else [4, 5, 6, 3]
                for kc in range(4):
                    op("pe", lambda e, kc=kc, U=U, bk=banks[0]: e.matmul(pbk[bk][:, :], lhsT=U[:, kc * 128:(kc + 1) * 128], rhs=oaT[:, kc, :], start=(kc == 0), stop=(kc == 3)),
                       r=skc + ["oaT%d" % q for q in range(4)], w=[BK[banks[0]]])
                for kc in range(4):
                    op("pe", lambda e, kc=kc, U=U, bk=banks[1]: e.matmul(pbk[bk][:, :], lhsT=U[:, 512 + kc * 128:512 + (kc + 1) * 128], rhs=obT[:, kc, :], start=(kc == 0), stop=(kc == 3)),
                       r=skc + ["obT%d" % q for q in range(4)], w=[BK[banks[1]]])
                for ab in range(2):
                    for kc in range(8):
                        op("pe", lambda e, kc=kc, U=U, ab=ab, bk=banks[2 + ab]: e.matmul(pbk[bk][:, :], lhsT=U[:, 1024 + ab * 1024 + kc * 128:1024 + ab * 1024 + (kc + 1) * 128],
                                                                                         rhs=hnT[:, kc, :], start=(kc == 0), stop=(kc == 7)),
                           r=skc + ["hnT%d" % q for q in range(4)], w=[BK[banks[2 + ab]]])
                    op("act", lambda e, ab=ab, sl=sl, bk=banks[2 + ab], c=c: e.activation(tAB[sl][:, ab, :], pbk[bk][:, :], AF.Tanh, scale=0.5, bias=hbg[:, ab * 8 + c:ab * 8 + c + 1]),
                       r=[BK[banks[2 + ab]], "hbg"], w=["tAB%d%d" % (sl, ab)])
                    op("dve", lambda e, ab=ab, sl=sl, bk=banks[ab]: e.scalar_tensor_tensor(mAB[sl][:, ab, :], tAB[sl][:, ab, :], 1.0, pbk[bk][:, :], ALU.add, ALU.mult),
                       r=["tAB%d%d" % (sl, ab), BK[banks[ab]]], w=["mAB%d%d" % (sl, ab)])
                op("pool", lambda e, sl=sl, c=c: e.tensor_tensor(mixedT[:, c, :], mAB[sl][:, 0, :], mAB[sl][:, 1, :], ALU.add),
                   r=["mAB%d0" % sl, "mAB%d1" % sl], w=["mixedT"])
            for j in range(4):
                for half in range(2):
                    bk = half
                    for c in range(8):
                        op("pe", lambda e, c=c, j=j, half=half, bk=bk: e.matmul(pbk[bk][:, :], lhsT=mixedT[:, c, j * 128:(j + 1) * 128],
                                                                                rhs=w_out_sb[:, c, half * 512:(half + 1) * 512], start=(c == 0), stop=(c == 7)),
                           r=["mixedT", "w_out_sb"], w=[BK[bk]])
                    op("dve", lambda e, j=j, half=half, bk=bk: e.scalar_tensor_tensor(xh[:, j, half * 512:(half + 1) * 512], pbk[bk][:, :], 0.5,
                                                                                      xh[:, j, half * 512:(half + 1) * 512], ALU.mult, ALU.add),
                       r=[BK[bk], "xh%d" % j], w=["xh%d" % j])
                op("act", lambda e, j=j: e.activation(junk[:, :], xh[:, j, :], AF.Square, accum_out=stat[:, 16 + j:17 + j]), r=["xh%d" % j], w=["junk", "stat"])
            rstd_from(stat[:, 16:20], stat[:, 16:20], 1.0 / D, ["stat"])
            for j in range(4):
                norm_T(xh[:, j, :], stat[:, 16 + j:17 + j], C_GFFN, hn2T[:, :, j * 128:(j + 1) * 128], ["xh%d" % j, "stat"], ["hn2T"])

        def phase_DE(s, b):
            sflat = stream[:, :, :].rearrange("p a e -> p (a e)")
            sl3 = [sflat[:, k * FF_UNIT:(k + 1) * FF_UNIT] for k in range(3)]
            sk3 = [["sq0", "sq1"], ["sq2", "sq3"], ["sq4", "sq5"]]
            for q in range(2):
                dma("sp", wdb[q][:, :, :], wd_s[q].rearrange("p (k f) -> p k f", k=NFC), r=["scr"], w=["wdb%d" % q], sem="s_wd%d" % q)
            for fc in range(NFC):
                sl = fc % 3
                pb2 = fc % 2
                dma("sp", sl3[sl], ff_s[fc], r=["scr"], w=sk3[sl], sem="s_sf%d" % sl)
                V = sl3[sl]
                bg, bu = 2 * pb2, 2 * pb2 + 1
                for kc in range(8):
                    op("pe", lambda e, kc=kc, V=V, bg=bg: e.matmul(pbk[bg][:, :], lhsT=V[:, kc * 128:(kc + 1) * 128], rhs=hn2T[:, kc, :], start=(kc == 0), stop=(kc == 7)),
                       r=sk3[sl] + ["hn2T"], w=[BK[bg]])
                for kc in range(8):
                    op("pe", lambda e, kc=kc, V=V, bu=bu: e.matmul(pbk[bu][:, :], lhsT=V[:, 1024 + kc * 128:1024 + (kc + 1) * 128], rhs=hn2T[:, kc, :], start=(kc == 0), stop=(kc == 7)),
                       r=sk3[sl] + ["hn2T"], w=[BK[bu]])
                op("act", lambda e, pb2=pb2, bg=bg: e.activation(tg[pb2], pbk[bg][:, :], AF.Tanh, scale=0.5), r=[BK[bg]], w=["tAB%d0" % pb2])
                op("dve", lambda e, pb2=pb2, bg=bg: e.scalar_tensor_tensor(a1[pb2], tg[pb2], 1.0, pbk[bg][:, :], ALU.add, ALU.mult), r=["tAB%d0" % pb2, BK[bg]], w=["mAB%d0" % pb2])
                op("dve", lambda e, pb2=pb2, bu=bu, fc=fc: e.tensor_tensor(actT[:, fc, :], a1[pb2], pbk[bu][:, :], ALU.mult), r=["mAB%d0" % pb2, BK[bu]], w=["actT"])
            for q in range(8):
                ws = q % 2
                if q >= 2:
                    dma("sp", wdb[ws][:, :, :], wd_s[q].rearrange("p (k f) -> p k f", k=NFC), r=["scr"], w=["wdb%d" % ws], sem="s_wd%d" % ws)
                for j in range(4):
                    bk = 4 + (q * 4 + j) % 3
                    for fc in range(NFC):
                        op("pe", lambda e, fc=fc, j=j, ws=ws, bk=bk: e.matmul(pbk[bk][:, 0:128], lhsT=actT[:, fc, j * 128:(j + 1) * 128], rhs=wdb[ws][:, fc, :],
                                                                              start=(fc == 0), stop=(fc == NFC - 1)), r=["actT", "wdb%d" % ws], w=[BK[bk]])
                    op("dve", lambda e, j=j, q=q, bk=bk: e.scalar_tensor_tensor(xh[:, j, q * 128:(q + 1) * 128], pbk[bk][:, 0:128], 0.5,
                                                                                 xh[:, j, q * 128:(q + 1) * 128], ALU.mult, ALU.add),
                       r=[BK[bk], "xh%d" % j], w=["xh%d" % j])
            for j in range(4):
                op("act", lambda e, j=j: e.activation(junk[:, :], xh[:, j, :], AF.Square, accum_out=stat[:, 24 + j:25 + j]), r=["xh%d" % j], w=["junk", "stat"])
            rstd_from(stat[:, 24:28], stat[:, 24:28], 1.0 / D, ["stat"])
            for j in range(4):
                t = 4 * b + j + 1
                eng = "dve" if j % 2 == 0 else "pool"
                if eng == "dve":
                    op("dve", lambda e, j=j: e.scalar_tensor_tensor(xh[:, j, :], xh[:, j, :], stat[:, 24 + j:25 + j], cst[:, C_GFIN:C_GFIN + D], ALU.mult, ALU.mult),
                       r=["xh%d" % j, "stat", "cst"], w=["xh%d" % j])
                else:
                    op("pool", lambda e, j=j: e.tensor_scalar(xh[:, j, :], xh[:, j, :], stat[:, 24 + j:25 + j], None, ALU.mult), r=["xh%d" % j, "stat"], w=["xh%d" % j])
                    op("pool", lambda e, j=j: e.tensor_tensor(xh[:, j, :], xh[:, j, :], cst[:, C_GFIN:C_GFIN + D], ALU.mult), r=["xh%d" % j, "cst"], w=["xh%d" % j])
                dma("sp", y_d[s, (t - 1) * 128:t * 128, :], xh[:, j, :], r=["xh%d" % j], w=["y"], sem="s_x%d" % j)

        for s in range(nseq):
            try:
                tile_A(s, 0)
            except _Stop:
                P.finish([])
                return nc
            if stop == 4:
                P.finish([])
                return nc
            for b in range(nblk // 4):
                tl = [4 * b + j + 1 for j in range(4)]
                P.phase = "A"
                tile_A(s, tl[0])
                P.phase = "B"
                tile_B(s, tl[0])
                pend.extend(bis_steps(tl[0]))
                P.phase = "A"
                tile_A(s, tl[1])
                P.phase = "B"
                while pend:
                    pend.pop(0)()
                tile_B1post(s, tl[0])
                for ii in range(1, 4):
                    tile_B(s, tl[ii])
                    pend.extend(bis_steps(tl[ii]))
                    if ii + 1 < 4:
                        P.phase = "A"
                        tile_A(s, tl[ii + 1])
                        P.phase = "B"
                    rest = list(pend)
                    del pend[:]
                    tile_B2(s, tl[ii - 1], rest)
                    tile_B1post(s, tl[ii])
                tile_B2(s, tl[3], [])
                P.barrier()
                P.phase = "C"
                phase_C(s, b)
                P.phase = "DE"
                if stop == 8:
                    P.finish([])
                    return nc
                phase_DE(s, b)
                P.barrier()
        P.finish(["s_x0", "s_x1", "s_x2", "s_x3"])
    return nc


def _host_consts(inp):
    f32 = np.float32
    cst = np.zeros((128, NCONST), f32)
    cst[:, C_GATT:C_GATT + 8] = np.asarray(inp["attn_norm_g"], f32)[0].reshape(8, 128).T
    cst[:, C_GFFN:C_GFFN + 8] = np.asarray(inp["ffn_norm_g"], f32)[0].reshape(8, 128).T
    cst[:, C_BG:C_BG + 16] = np.asarray(inp["b_gates"], f32)[0].reshape(16, 128).T
    cst[:, C_SINK:C_SINK + 8] = np.asarray(inp["sinks"], f32)[0][HEADPOS][None, :]
    rb = np.asarray(inp["rel_bias"], f32)
    cst[:, C_BFAR:C_BFAR + 8] = rb[31, 8:16][None, :]
    cst[:, C_GQ:C_GQ + 256] = np.asarray(inp["q_norm_g"], f32)[0][None, :]
    cst[:, C_GKV:C_GKV + 128] = np.asarray(inp["kv_norm_g"], f32)[0][None, :]
    cst[:, C_LNG:C_LNG + 64] = np.asarray(inp["idx_k_ln_g"], f32)[0][None, :]
    cst[:, C_LNB:C_LNB + 64] = np.asarray(inp["idx_k_ln_b"], f32)[0][None, :]
    cst[:, C_GFIN:C_GFIN + D] = np.asarray(inp["final_norm_g"], f32)[None, :]
    qi = np.arange(128)[:, None]
    ki = np.arange(128)[None, :]
    negm = np.zeros((128, 2, 128), f32)
    negm[:, 0, :] = np.where(ki <= qi, 0.0, -1e30)
    negm[:, 1, :] = np.where(ki >= NPAD, 0.0, -1e30)
    cst[:, C_NEGM:C_NEGM + 256] = negm.reshape(128, 256)
    kk = np.arange(128)[:, None]
    qq = np.arange(128)[None, :]
    maskA = np.zeros((128, 3, 128), f32)
    maskA[:, 0, :] = (qq >= kk)
    maskA[:, 1, :] = (qq < kk)
    maskA[:, 2, :] = (qq < kk) & (kk >= NPAD)
    cst[:, C_MASKA:C_MASKA + 384] = maskA.reshape(128, 384)
    cst[:, C_CTAB:C_CTAB + 16] = (0.5 ** np.arange(1, 17, dtype=np.float64)).astype(f32)[None, :]
    dist_d = np.maximum(qq - kk, 0)
    dist_s = 128 + qq - kk
    bd = _t5_bucket_np(dist_d)
    bs = _t5_bucket_np(dist_s)
    biasT = np.zeros((128, 4, 8, 128), f32)
    for pos in range(8):
        biasT[:, 0, pos, :] = rb[bd, HEADPOS[pos]]
        biasT[:, 1, pos, :] = rb[bs, HEADPOS[pos]]
        biasT[:, 2, pos, :] = rb[bd, 8 + pos]
        biasT[:, 3, pos, :] = rb[bs, 8 + pos]
    return cst, biasT.reshape(128, 4 * 8 * 128)


def _in_maps(inp, ncores, nseq):
    f32 = np.float32
    cst, biasT = _host_consts(inp)
    wa = np.asarray(inp["w_branch_a"], f32)[0].reshape(8, 64, D)[HEADPOS].reshape(512, D)
    common = {
        "meta": np.ascontiguousarray(np.asarray(inp["meta_tokens"], f32)),
        "w_in": np.ascontiguousarray(np.asarray(inp["w_in"], f32)[0]),
        "w_uq": np.ascontiguousarray(np.asarray(inp["w_uq"], f32)[0]),
        "w_uk": np.ascontiguousarray(np.asarray(inp["w_uk"], f32)[0].reshape(128, 512)),
        "w_uv": np.ascontiguousarray(np.asarray(inp["w_uv"], f32)[0].reshape(128, 512)),
        "w_a": np.ascontiguousarray(wa),
        "w_b": np.ascontiguousarray(np.asarray(inp["w_branch_b"], f32)[0]),
        "w_out": np.ascontiguousarray(np.asarray(inp["w_out"], f32)[0]),
        "wg": np.ascontiguousarray(np.asarray(inp["w_ffn_gate"], f32)[0]),
        "wu": np.ascontiguousarray(np.asarray(inp["w_ffn_up"], f32)[0]),
        "wd": np.ascontiguousarray(np.asarray(inp["w_ffn_down"], f32)[0]),
        "cst": cst, "biasT": biasT, "ident": np.eye(128, dtype=f32),
    }
    x = np.asarray(inp["x"], f32)
    maps = []
    for c in range(ncores):
        m = dict(common)
        m["x"] = np.ascontiguousarray(x[c * nseq:(c + 1) * nseq])
        maps.append(m)
    return maps


def kernel(**inputs):
    ncores = 8
    x = inputs["x"]
    bsz, seq, _ = x.shape
    nseq = bsz // ncores
    nblk = seq // 128
    topk = min(256, seq // 4)
    nc = build_nc(nseq, nblk, topk)
    maps = _in_maps(inputs, ncores, nseq)
    res = run_bass_kernel_spmd(nc, maps, core_ids=list(range(ncores)))
    out = np.concatenate([np.asarray(r["y"], np.float32) for r in res.results], axis=0)
    return out
```

```python
import numpy as np
from contextlib import ExitStack
import concourse.bass as bass
import concourse.mybir as mybir
from concourse.bass_utils import run_bass_kernel_spmd

F32 = mybir.dt.float32
BF16 = mybir.dt.bfloat16
ALU = mybir.AluOpType
AF = mybir.ActivationFunctionType
AX = mybir.AxisListType


class _Op:
    __slots__ = ("idx", "eng", "fn", "waits", "dma_sem", "flagged", "sigval", "phase")


class Prog:
    ENGS = ("pe", "act", "dve", "pool", "sp")

    def __init__(self, nc, es):
        self.nc, self.es = nc, es
        self.ops = []
        self.eng_ops = {e: [] for e in self.ENGS}
        self.last_w = {}
        self.readers = {}
        self.dma_count = {}
        self.tail_fn = None
        self.phase = "setup"

    def sb(self, name, shape, dtype):
        return self.es.enter_context(self.nc.sbuf_tensor("sb_" + name, list(shape), dtype))

    def ps(self, name, shape, dtype):
        return self.es.enter_context(self.nc.psum_tensor("ps_" + name, list(shape), dtype))

    def _add(self, eng, fn, r, w, dma_sem):
        o = _Op()
        o.idx = len(self.ops)
        o.eng, o.fn, o.dma_sem = eng, fn, dma_sem
        o.phase = self.phase
        o.flagged = False
        o.sigval = 0
        deps = {}
        for k in r:
            d = self.last_w.get(k)
            if d is not None:
                deps[d] = True
        for k in w:
            d = self.last_w.get(k)
            if d is not None:
                deps.setdefault(d, False)
            for d in self.readers.get(k, ()):
                deps.setdefault(d, False)
        waits = []
        for d, raw in deps.items():
            do = self.ops[d]
            if do.dma_sem is not None:
                waits.append(("dma", do.dma_sem, 16 * self.dma_count[do.dma_sem]))
            elif do.eng == eng:
                if eng == "pe":
                    continue
                do.flagged = True
                waits.append(("eng", d))
            else:
                do.flagged = True
                waits.append(("eng", d))
        o.waits = waits
        for k in r:
            self.readers.setdefault(k, []).append(o.idx)
        for k in w:
            self.last_w[k] = o.idx
            self.readers[k] = []
        if dma_sem is not None:
            self.dma_count[dma_sem] = self.dma_count.get(dma_sem, 0) + 1
        self.ops.append(o)
        self.eng_ops[eng].append(o)
        return o

    def op(self, eng, fn, r=(), w=()):
        return self._add(eng, fn, r, w, None)

    def dma(self, eng, out, in_, r=(), w=(), sem=None, **kw):
        def fn(e, out=out, in_=in_, kw=kw):
            return e.dma_start(out=out, in_=in_, **kw)
        return self._add(eng, fn, r, w, sem)

    def barrier(self, dma_sems=()):
        last = {e: (self.eng_ops[e][-1] if self.eng_ops[e] else None) for e in self.ENGS}
        for e in self.ENGS:
            o = _Op()
            o.idx = len(self.ops)
            o.eng, o.fn, o.dma_sem = e, None, None
            o.flagged = False
            o.sigval = 0
            o.waits = []
            for e2 in self.ENGS:
                lo = last[e2]
                if e2 == e or lo is None:
                    continue
                while lo is not None and lo.fn is None:
                    k = self.eng_ops[e2].index(lo)
                    lo = self.eng_ops[e2][k - 1] if k > 0 else None
                if lo is None:
                    continue
                if lo.dma_sem is None:
                    lo.flagged = True
                    o.waits.append(("eng", lo.idx))
            for sname, cnt in self.dma_count.items():
                o.waits.append(("dma", sname, 16 * cnt))
            self.ops.append(o)
            self.eng_ops[e].append(o)

    def finish(self, out_sems):
        if self.tail_fn is not None:
            self.tail_fn()
        o = _Op()
        o.idx = len(self.ops)
        o.eng, o.fn, o.dma_sem = "sp", None, None
        o.flagged = False
        o.sigval = 0
        o.waits = [("dma", s, 16 * self.dma_count[s]) for s in out_sems]
        self.ops.append(o)
        self.eng_ops["sp"].append(o)
        self.emit()

    def emit(self):
        nc = self.nc
        sem_eng = {e: self.es.enter_context(nc.semaphore("sem_" + e)) for e in self.ENGS}
        sem_dma = {s: self.es.enter_context(nc.semaphore("sd_" + s)) for s in self.dma_count}
        for e in self.ENGS:
            c = 0
            for o in self.eng_ops[e]:
                if o.flagged:
                    c += 1
                    o.sigval = c

        def run(ename, e):
            seen = {}
            for o in self.eng_ops[ename]:
                for wt in o.waits:
                    if wt[0] == "dma":
                        key, sem, val = wt[1], sem_dma[wt[1]], wt[2]
                    else:
                        do = self.ops[wt[1]]
                        key, sem, val = "E" + do.eng, sem_eng[do.eng], do.sigval
                    if seen.get(key, 0) >= val:
                        continue
                    seen[key] = val
                    e.wait_ge(sem, val)
                if o.fn is None:
                    continue
                inst = o.fn(e)
                if o.dma_sem is not None:
                    inst.then_inc(sem_dma[o.dma_sem], 16)
                elif o.flagged:
                    inst.then_inc(sem_eng[ename], 1)

        with nc.Block() as block:
            @block.tensor
            def _(e):
                run("pe", e)

            @block.scalar
            def _(e):
                run("act", e)

            @block.vector
            def _(e):
                run("dve", e)

            @block.gpsimd
            def _(e):
                run("pool", e)

            @block.sync
            def _(e):
                run("sp", e)


D = 1024
DIN = 3524
NRES = 1476
DFF = 2816
NFC = 22
NPAD = 112
EPS = 1e-6
HEADPOS = [0, 2, 1, 3, 4, 6, 5, 7]

C_GATT, C_GFFN, C_BG, C_SINK, C_BFAR = 0, 8, 16, 32, 40
C_GQ, C_GKV, C_LNG, C_LNB, C_GFIN, C_NEGM, C_MASKA = 48, 304, 432, 496, 560, 1584, 1840
C_CTAB = 1840 + 384
NCONST = C_CTAB + 16

PC_UNIT = 3072
FF_UNIT = 2048
WD_UNIT = NFC * 128
S_PC = 0
S_FF = S_PC + 8 * 128 * PC_UNIT
S_WD = S_FF + NFC * 128 * FF_UNIT
S_TOT = S_WD + 8 * 128 * WD_UNIT


def _t5_bucket_np(dist):
    d = np.maximum(dist, 1).astype(np.float32)
    large = 16 + (np.log(d / np.float32(16)) / np.float32(np.log(128 / 16)) * np.float32(16)).astype(np.int32)
    large = np.minimum(large, 31)
    return np.where(dist < 16, dist, large)


class _Stop(Exception):
    pass


def build_nc(nseq, nblk, topk, n_iter=8, stop=0):
    nc = bass.Bass("TRN2", target_bir_lowering=False)
    T = nblk * 128
    NKB = nblk + 1
    NK = NKB * 128

    def din(name, shape, dt=F32):
        return nc.dram_tensor(name, list(shape), dt, kind="ExternalInput").ap()

    x_d = din("x", [nseq, T, D])
    meta_d = din("meta", [16, D])
    win_d = din("w_in", [D, DIN])
    wuq_d = din("w_uq", [256, 512])
    wuk_d = din("w_uk", [128, 512])
    wuv_d = din("w_uv", [128, 512])
    wa_d = din("w_a", [512, D])
    wb_d = din("w_b", [512, D])
    wout_d = din("w_out", [D, D])
    wg_d = din("wg", [D, DFF])
    wu_d = din("wu", [D, DFF])
    wd_d = din("wd", [DFF, D])
    cst_d = din("cst", [128, NCONST])
    bias_d = din("biasT", [128, 4 * 8 * 128])
    id_d = din("ident", [128, 128])
    y_d = nc.dram_tensor("y", [nseq, T, D], F32, kind="ExternalOutput").ap()
    scr = nc.dram_tensor("wscr", [S_TOT], BF16, kind="ExternalOutput").ap()

    def scr_view(off, n_units, unit):
        return scr[off:off + n_units * 128 * unit].rearrange("(u p e) -> u p e", p=128, e=unit)

    pc_s = scr_view(S_PC, 8, PC_UNIT)
    ff_s = scr_view(S_FF, NFC, FF_UNIT)
    wd_s = scr_view(S_WD, 8, WD_UNIT)

    with ExitStack() as es:
        P = Prog(nc, es)
        sb, op, dma = P.sb, P.op, P.dma

        w_in_sb = sb("w_in_sb", [128, 8, NRES], BF16)
        w_out_sb = sb("w_out_sb", [128, 8, D], BF16)
        w_uq_sb = sb("w_uq_sb", [128, 2, 512], BF16)
        w_ukTp = sb("w_ukTp", [128, 8, 128], BF16)
        w_uvp = sb("w_uvp", [128, 8, 128], BF16)
        cst = sb("cst", [128, NCONST], F32)
        ident = sb("ident_sb", [128, 128], BF16)
        EA = sb("EA", [128, 3, 8, 128], BF16)
        EB = sb("EB", [128, 2, 8, 128], BF16)
        hbg = sb("hbg", [128, 16], F32)
        nbfar = sb("nbfar", [128, 8], F32)
        esink = sb("esink", [128, 8], F32)
        ckvT = sb("ckvT", [128, NK], BF16)
        ckv1 = sb("ckv1", [128, NKB, 130], BF16)
        ikT = sb("ikT", [128, NK], BF16)
        akTp = sb("akTp", [128, 6, 4, 128], BF16)
        av1 = sb("av1", [128, 6, 2, 66], BF16)
        xh = sb("xh", [128, 4, D], F32)
        hnT = sb("hnT", [128, 8, 512], BF16)
        oaT = sb("oaT", [128, 4, 512], BF16)
        obT = sb("obT", [128, 4, 512], BF16)
        mixedT = sb("mixedT", [128, 8, 512], BF16)
        xb = sb("xb", [128, D], BF16)
        tmpf = sb("tmpf", [128, 512], F32)
        stat = sb("stat", [128, 32], F32)
        stat2 = sb("stat2", [128, 16], F32)
        bis = sb("bis", [128, 8], F32)
        iw_s = sb("iw_s", [128, 4, 4], F32)
        stream = sb("stream", [128, 2, PC_UNIT], BF16)
        akpad = sb("akpad", [128, 4, 128], BF16)
        iqpad = sb("iqpad", [128, 4, 128], BF16)
        ikdup = sb("ikdup", [128, 128], BF16)
        aq_tok = sb("aq_tok", [128, 512], BF16)
        qn_tok = sb("qn_tok", [128, 256], BF16)
        qnT = sb("qnT", [128, 2, 128], BF16)
        qT = sb("qT", [128, 4, 128], BF16)
        junk = sb("junk", [128, D], BF16)
        dtab = sb("dtab", [128, 16], F32)
        dtab2n = sb("dtab2n", [128, 16], F32)

        ARENA_F32 = (nc.sbuf_bytes_remaining - 256) // 4
        arena = sb("arena", [128, ARENA_F32], F32)

        class Carve:
            def __init__(self):
                self.off = 0

            def f32(self, n):
                a = arena[:, self.off:self.off + n]
                self.off += n
                assert self.off <= ARENA_F32, self.off
                return a

            def bf(self, n):
                assert n % 2 == 0
                return self.f32(n // 2).bitcast(BF16)

        cv = Carve()
        aqT = cv.bf(4 * 4 * 128).rearrange("p (j c q) -> p j c q", j=4, c=4)
        iqTp = cv.bf(4 * 4 * 128).rearrange("p (j c q) -> p j c q", j=4, c=4)
        qabsT = cv.bf(4 * 8 * 128).rearrange("p (j h q) -> p j h q", j=4, h=8)
        scores = cv.f32(NK)
        mask = cv.bf(NK)
        maskT = [cv.bf(NK).rearrange("p (k q) -> p k q", q=128) for _ in range(2)]
        eB = [cv.bf(1024).rearrange("p (h q) -> p h q", h=8) for _ in range(2)]
        pB = [cv.bf(1024).rearrange("p (h q) -> p h q", h=8) for _ in range(2)]
        lat = cv.bf(1024).rearrange("p (h q) -> p h q", h=8)
        latT = cv.bf(1024).rearrange("p (h q) -> p h q", h=8)
        oa_tok = cv.bf(512)
        rbuf = [cv.f32(512) for _ in range(2)]
        off_ab = cv.off
        cv.off = 0
        tAB = [cv.bf(1024).rearrange("p (a q) -> p a q", a=2) for _ in range(2)]
        mAB = [cv.f32(1024).rearrange("p (a q) -> p a q", a=2) for _ in range(2)]
        hn2T = cv.bf(8 * 512).rearrange("p (k q) -> p k q", k=8)
        actT = cv.bf(NFC * 512).rearrange("p (k q) -> p k q", k=NFC)
        wdb = [cv.bf(WD_UNIT).rearrange("p (k q) -> p k q", k=NFC) for _ in range(2)]
        tg = [tAB[i][:, 0, :] for i in range(2)]
        a1 = [mAB[i][:, 0, :] for i in range(2)]
        stg_bf = arena[:, :].bitcast(BF16)

        pend = []
        _sk = [0]

        def skey():
            _sk[0] += 1
            return "scrw%d" % _sk[0]

        def ck(n):
            for _ in range(2):
                if pend:
                    pend.pop(0)()
            if stop == n:
                raise _Stop()

        def _tail():
            P.op("dve", lambda e: e.memset(stat2[:, 15:16], 0.0), r=["stat2"], w=["stat2"])
        P.tail_fn = _tail

        pbk = [P.ps("pb%d" % i, [128, 512], F32) for i in range(8)]
        pT = pbk[7][:, :].bitcast(BF16)
        BK = ["B%d" % i for i in range(8)]

        dma("sp", cst[:, :], cst_d, w=["cst"], sem="s_cst")
        dma("pool", ident[:, :], id_d, w=["ident"], sem="s_id")
        for kc in range(8):
            dma("pool", w_in_sb[:, kc, :], win_d[kc * 128:(kc + 1) * 128, 0:NRES], w=["w_in_sb"], sem="s_win")
        dma("pool", w_out_sb[:, :, :], wout_d.rearrange("(k p) n -> p k n", p=128), w=["w_out_sb"], sem="s_wout")
        dma("pool", w_uq_sb[:, :, :], wuq_d.rearrange("(k p) n -> p k n", p=128), w=["w_uq_sb"], sem="s_wuq")
        op("dve", lambda e: e.memset(w_uvp[:, :, :], 0.0), w=["w_uvp"])
        wuv_v = wuv_d.rearrange("c (h d) -> c h d", d=64)
        for hh in range(2):
            for h in range(hh, 8, 2):
                dma("pool", w_uvp[:, h, hh * 64:(hh + 1) * 64], wuv_v[:, h, :], r=[], w=["w_uvp"], sem="s_wuv")
        wuk_tmp = stg_bf[:, 14336:14848]
        dma("pool", wuk_tmp, wuk_d, w=["stg0"], sem="s_stg0")
        op("dve", lambda e: e.memset(w_ukTp[:, :, :], 0.0), w=["w_ukTp"])
        for pr in range(4):
            op("pe", lambda e, pr=pr: e.transpose(pT[:, pr * 128:(pr + 1) * 128], wuk_tmp[:, pr * 128:(pr + 1) * 128], ident[:, :]),
               r=["stg0", "ident"], w=["B7"])
        for pr in range(4):
            op("dve", lambda e, pr=pr: e.tensor_copy(w_ukTp[0:64, 2 * pr, :], pT[0:64, pr * 128:(pr + 1) * 128]), r=["B7"], w=["w_ukTp"])
            op("dve", lambda e, pr=pr: e.tensor_copy(w_ukTp[64:128, 2 * pr + 1, :], pT[64:128, pr * 128:(pr + 1) * 128]), r=["B7"], w=["w_ukTp"])
        op("dve", lambda e: e.tensor_scalar(hbg[:, :], cst[:, C_BG:C_BG + 16], 0.5, None, ALU.mult), r=["cst"], w=["hbg"])
        op("dve", lambda e: e.tensor_scalar(nbfar[:, :], cst[:, C_BFAR:C_BFAR + 8], -1.0, None, ALU.mult), r=["cst"], w=["nbfar"])
        op("act", lambda e: e.activation(esink[:, :], cst[:, C_SINK:C_SINK + 8], AF.Exp), r=["cst"], w=["esink"])
        op("dve", lambda e: e.memset(stat[:, :], 1.0), w=["stat"])
        op("dve", lambda e: e.memset(stat2[:, :], 1.0), w=["stat2"])
        op("dve", lambda e: e.memset(bis[:, :], 0.0), w=["bis"])
        op("pool", lambda e: e.memset(akpad[:, :, :], 0.0), w=["akpad"])
        op("pool", lambda e: e.memset(iqpad[:, :, :], 0.0), w=["iqpad"])
        op("pool", lambda e: e.memset(av1[:, :, :, 64:65], 1.0), w=["av1"])
        op("pool", lambda e: e.memset(ckv1[:, :, 128:129], 1.0), w=["ckv1"])
        if stop == 1:
            P.finish([])
            return nc
        btmp = arena[:, 0:4096].rearrange("p (v h q) -> p v h q", v=4, h=8)
        etmp = arena[:, 4096:4096 + 3072].rearrange("p (v h q) -> p v h q", v=3, h=8)
        dma("sp", arena[:, 0:4096], bias_d, w=["btmp"], sem="s_btmp")
        op("act", lambda e: e.activation(etmp[:, 0, :, :], btmp[:, 0, :, :], AF.Exp), r=["btmp"], w=["etmp0"])
        op("act", lambda e: e.activation(etmp[:, 1, :, :], btmp[:, 1, :, :], AF.Exp), r=["btmp"], w=["etmp1"])
        mA_ = cst[:, C_MASKA:C_MASKA + 384].rearrange("p (v q) -> p v q", v=3)

        def mbc(v):
            return mA_[:, v:v + 1, :].broadcast_to([128, 8, 128])

        op("dve", lambda e: e.tensor_tensor(EA[:, 0, :, :], etmp[:, 0, :, :], mbc(0), ALU.mult), r=["etmp0", "cst"], w=["EA"])
        op("dve", lambda e: e.tensor_tensor(EA[:, 1, :, :], etmp[:, 1, :, :], mbc(1), ALU.mult), r=["etmp1", "cst"], w=["EA"])
        op("dve", lambda e: e.tensor_tensor(EA[:, 2, :, :], etmp[:, 1, :, :], mbc(2), ALU.mult), r=["etmp1", "cst"], w=["EA"])
        for v in range(2):
            for h in range(8):
                op("act", lambda e, v=v, h=h: e.activation(etmp[:, 2, h, :], btmp[:, 2 + v, h, :], AF.Exp, bias=nbfar[:, h:h + 1]),
                   r=["btmp", "nbfar", "EB"], w=["etmp2"])
            if v == 0:
                op("dve", lambda e: e.tensor_tensor(EB[:, 0, :, :], etmp[:, 2, :, :], mbc(0), ALU.mult), r=["etmp2", "cst"], w=["EB"])
            else:
                op("dve", lambda e: e.tensor_copy(EB[:, 1, :, :], etmp[:, 2, :, :]), r=["etmp2"], w=["EB"])
        P.barrier()
        if stop == 2:
            P.finish([])
            return nc

        g_stg = stg_bf[:, 0:16384].rearrange("p (k n) -> p k n", k=8)
        a_stg = stg_bf[:, 16384:20480].rearrange("p (k n) -> p k n", k=4)
        b_stg = stg_bf[:, 20480:24576].rearrange("p (k n) -> p k n", k=4)
        for kc in range(8):
            dma("pool", g_stg[:, kc, :], win_d[kc * 128:(kc + 1) * 128, NRES:DIN], w=["stgA"], sem="s_stgA")
        dma("pool", a_stg, wa_d.rearrange("(k p) n -> p k n", p=128), w=["stgA"], sem="s_stgA")
        dma("pool", b_stg, wb_d.rearrange("(k p) n -> p k n", p=128), w=["stgA"], sem="s_stgA")
        for c in range(8):
            u = pc_s[c]
            dma("sp", u[:, 0:512].rearrange("p (k f) -> p k f", k=4), a_stg[:, :, c * 128:(c + 1) * 128], r=["stgA"], w=[skey()], sem="s_scr")
            dma("sp", u[:, 512:1024].rearrange("p (k f) -> p k f", k=4), b_stg[:, :, c * 128:(c + 1) * 128], r=["stgA"], w=[skey()], sem="s_scr")
            dma("sp", u[:, 1024:2048].rearrange("p (k f) -> p k f", k=8), g_stg[:, :, c * 128:(c + 1) * 128], r=["stgA"], w=[skey()], sem="s_scr")
            dma("sp", u[:, 2048:3072].rearrange("p (k f) -> p k f", k=8), g_stg[:, :, 1024 + c * 128:1024 + (c + 1) * 128], r=["stgA"], w=[skey()], sem="s_scr")
        d_stg = stg_bf[:, 0:NFC * 1024].rearrange("p (k n) -> p k n", k=NFC)
        for k0 in range(0, NFC, 11):
            dma("pool", d_stg[:, k0:k0 + 11, :], wd_d[k0 * 128:(k0 + 11) * 128, :].rearrange("(k p) n -> p k n", p=128),
                r=[], w=["stgA"], sem="s_stgA")
        for q in range(8):
            dma("sp", wd_s[q].rearrange("p (k f) -> p k f", k=NFC), d_stg[:, :, q * 128:(q + 1) * 128], r=["stgA"], w=[skey()], sem="s_scr")
        for hf in range(2):
            g2 = stg_bf[:, 0:8 * 1408].rearrange("p (k n) -> p k n", k=8)
            u2 = stg_bf[:, 8 * 1408:16 * 1408].rearrange("p (k n) -> p k n", k=8)
            for kc in range(8):
                dma("pool", g2[:, kc, :], wg_d[kc * 128:(kc + 1) * 128, hf * 1408:(hf + 1) * 1408], r=[], w=["stgA"], sem="s_stgA")
                dma("pool", u2[:, kc, :], wu_d[kc * 128:(kc + 1) * 128, hf * 1408:(hf + 1) * 1408], r=[], w=["stgA"], sem="s_stgA")
            for i in range(11):
                u = ff_s[hf * 11 + i]
                dma("sp", u[:, 0:1024].rearrange("p (k f) -> p k f", k=8), g2[:, :, i * 128:(i + 1) * 128], r=["stgA"], w=[skey()], sem="s_scr")
                dma("sp", u[:, 1024:2048].rearrange("p (k f) -> p k f", k=8), u2[:, :, i * 128:(i + 1) * 128], r=["stgA"], w=[skey()], sem="s_scr")
        P.barrier(dma_sems=["s_scr", "s_stgA"])
        if stop == 3:
            P.finish([])
            return nc

        def rstd_from(out_ap, in_ap, scale, keys):
            op("dve", lambda e: e.tensor_scalar(out_ap, in_ap, scale, EPS, ALU.mult, ALU.add), r=keys, w=keys)
            op("act", lambda e: e.activation(out_ap, out_ap, AF.Sqrt), r=keys, w=keys)
            op("dve", lambda e: e.reciprocal(out_ap, out_ap), r=keys, w=keys)

        def norm_T(src_ap, rstd_ap, gcol, dstT, skeys, dkeys):
            op("dve", lambda e: e.tensor_scalar(xb[:, :], src_ap, rstd_ap, None, ALU.mult), r=skeys, w=["xb"])
            for kc in range(8):
                op("pe", lambda e, kc=kc: e.transpose(pT[:, kc * 128:(kc + 1) * 128], xb[:, kc * 128:(kc + 1) * 128], ident[:, :]),
                   r=["xb", "ident"], w=["B7"])
            gb = cst[:, gcol:gcol + 8].unsqueeze(2).broadcast_to([128, 8, 128])
            op("dve", lambda e: e.tensor_tensor(dstT, pT[:, :].rearrange("p (k q) -> p k q", k=8), gb, ALU.mult),
               r=["B7", "cst"], w=dkeys)

        def tile_A(s, t):
            j = (t - 1) % 4 if t >= 1 else 0
            slot = t % 6
            xk = "xh%d" % j
            if t == 0:
                op("pool", lambda e: e.memset(xh[:, 0, :], 0.0), w=[xk])
                dma("sp", xh[NPAD:128, 0, :], meta_d, w=[xk], sem="s_x0")
            else:
                dma("sp", xh[:, j, :], x_d[s, (t - 1) * 128:t * 128, :], w=[xk], sem="s_x%d" % j)
            op("act", lambda e: e.activation(junk[:, :], xh[:, j, :], AF.Square, accum_out=stat[:, 0:1]), r=[xk], w=["junk", "stat"])
            rstd_from(stat[:, 0:1], stat[:, 0:1], 1.0 / D, ["stat"])
            ck(41)
            hT = hnT[:, :, j * 128:(j + 1) * 128]
            norm_T(xh[:, j, :], stat[:, 0:1], C_GATT, hT, [xk, "stat"], ["hnT%d" % j])
            ck(42)
            cols = [(0, 512), (512, 1024), (1024, NRES)]
            if t == 0:
                cols = cols[1:]
            for bi, (c0, c1) in enumerate(cols):
                bk = bi if t >= 1 else bi + 1
                for kc in range(8):
                    op("pe", lambda e, kc=kc, c0=c0, c1=c1, bk=bk: e.matmul(pbk[bk][:, 0:c1 - c0], lhsT=hnT[:, kc, j * 128:(j + 1) * 128],
                                                                          rhs=w_in_sb[:, kc, c0:c1], start=(kc == 0), stop=(kc == 7)),
                       r=["hnT%d" % j, "w_in_sb"], w=[BK[bk]])
            ck(43)
            for g in range(2):
                src = pbk[1][:, g * 64:(g + 1) * 64]
                op("act", lambda e, g=g, src=src: e.activation(akpad[:, 2 * g, 0:64], src, AF.Copy), r=["B1"], w=["akpad"])
                op("act", lambda e, g=g, src=src: e.activation(akpad[:, 2 * g + 1, 64:128], src, AF.Copy), r=["B1"], w=["akpad"])
            ck(4311)
            for g in range(2):
                op("act", lambda e, g=g: e.activation(av1[:, slot, g, 0:64], pbk[1][:, 128 + g * 64:128 + (g + 1) * 64], AF.Copy), r=["B1"], w=["av1"])
            ck(431)
            op("act", lambda e: e.activation(tmpf[:, 256:384], pbk[2][:, 0:128], AF.Square, accum_out=stat[:, 2:3]), r=["B2"], w=["tmpf_kv", "stat"])
            op("dve", lambda e: e.tensor_scalar(junk[:, 0:64], pbk[2][:, 384:448], 1.0 / 64, None, ALU.mult, ALU.add, accum_out=stat[:, 3:4]),
               r=["B2"], w=["junk", "stat"])
            op("dve", lambda e: e.tensor_scalar(tmpf[:, 384:448], pbk[2][:, 384:448], stat[:, 3:4], None, ALU.subtract), r=["B2", "stat"], w=["tmpf_ik"])
            op("act", lambda e: e.activation(junk[:, 0:64], tmpf[:, 384:448], AF.Square, accum_out=stat[:, 4:5]), r=["tmpf_ik"], w=["junk", "stat"])
            if t >= 1:
                op("act", lambda e: e.activation(junk[:, 0:256], pbk[1][:, 256:512], AF.Square, accum_out=stat[:, 1:2]), r=["B1"], w=["junk", "stat"])
            ck(432)
            op("dve", lambda e: e.tensor_scalar(stat[:, 1:2], stat[:, 1:2], 0.5, None, ALU.mult), r=["stat"], w=["stat"])
            op("dve", lambda e: e.tensor_scalar(stat[:, 4:5], stat[:, 4:5], 2.0, None, ALU.mult), r=["stat"], w=["stat"])
            op("dve", lambda e: e.tensor_copy(stat[:, 8:9], stat[:, 1:2]), r=["stat"], w=["stat"])
            op("dve", lambda e: e.tensor_copy(stat[:, 9:10], stat[:, 2:3]), r=["stat"], w=["stat"])
            op("dve", lambda e: e.tensor_copy(stat[:, 10:11], stat[:, 4:5]), r=["stat"], w=["stat"])
            rstd_from(stat[:, 8:11], stat[:, 8:11], 1.0 / 128, ["stat"])
            ck(433)
            op("dve", lambda e: e.scalar_tensor_tensor(ckv1[:, t, 0:128], pbk[2][:, 0:128], stat[:, 9:10], cst[:, C_GKV:C_GKV + 128], ALU.mult, ALU.mult),
               r=["B2", "stat", "cst"], w=["ckv1"])
            op("dve", lambda e: e.scalar_tensor_tensor(tmpf[:, 448:512], tmpf[:, 384:448], stat[:, 10:11], cst[:, C_LNG:C_LNG + 64], ALU.mult, ALU.mult),
               r=["tmpf_ik", "stat", "cst"], w=["junk2"])
            op("dve", lambda e: e.tensor_tensor(ikdup[:, 0:64], tmpf[:, 448:512], cst[:, C_LNB:C_LNB + 64], ALU.add), r=["junk2", "cst"], w=["ikdup"])
            op("dve", lambda e: e.tensor_copy(ikdup[:, 64:128], ikdup[:, 0:64]), r=["ikdup"], w=["ikdup"])
            ck(44)
            for v in range(4):
                op("pe", lambda e, v=v: e.transpose(pT[:, v * 128:(v + 1) * 128], akpad[:, v, :], ident[:, :]), r=["akpad", "ident"], w=["B7"])
            op("pe", lambda e: e.transpose(pT[:, 512:640], ckv1[:, t, 0:128], ident[:, :]), r=["ckv1", "ident"], w=["B7"])
            op("pe", lambda e: e.transpose(pT[:, 640:768], ikdup[:, :], ident[:, :]), r=["ikdup", "ident"], w=["B7"])
            ck(45)
            op("act", lambda e: e.activation(akTp[:, slot, :, :], pT[:, 0:512].rearrange("p (v q) -> p v q", v=4), AF.Copy), r=["B7"], w=["akTp"])
            ck(46)
            op("act", lambda e: e.activation(ckvT[:, t * 128:(t + 1) * 128], pT[:, 512:640], AF.Copy), r=["B7"], w=["ckvT"])
            op("act", lambda e: e.activation(ikT[:, t * 128:(t + 1) * 128], pT[:, 640:768], AF.Copy), r=["B7"], w=["ikT"])
            if t == 0:
                return
            op("act", lambda e: e.activation(aq_tok[:, :], pbk[0][:, :], AF.Copy), r=["B0"], w=["aq_tok"])
            op("dve", lambda e: e.scalar_tensor_tensor(qn_tok[:, :], pbk[1][:, 256:512], stat[:, 8:9], cst[:, C_GQ:C_GQ + 256], ALU.mult, ALU.mult),
               r=["B1", "stat", "cst"], w=["qn_tok"])
            for h in range(4):
                hh = h % 2
                op("act", lambda e, h=h, hh=hh: e.activation(iqpad[:, h, hh * 64:(hh + 1) * 64], pbk[2][:, 128 + h * 64:128 + (h + 1) * 64], AF.Copy),
                   r=["B2"], w=["iqpad"])
            op("act", lambda e: e.activation(iw_s[:, j, :], pbk[2][:, 448:452], AF.Copy, scale=1.0 / 16), r=["B2"], w=["iw_s"])
            for c in range(4):
                op("pe", lambda e, c=c: e.transpose(pT[:, c * 128:(c + 1) * 128], aq_tok[:, c * 128:(c + 1) * 128], ident[:, :]), r=["aq_tok", "ident"], w=["B7"])
            for c in range(2):
                op("pe", lambda e, c=c: e.transpose(pT[:, 512 + c * 128:512 + (c + 1) * 128], qn_tok[:, c * 128:(c + 1) * 128], ident[:, :]),
                   r=["qn_tok", "ident"], w=["B7"])
            op("act", lambda e: e.activation(aqT[:, j, :, :], pT[:, 0:512].rearrange("p (c q) -> p c q", c=4), AF.Copy), r=["B7"], w=["aqT%d" % j])
            op("act", lambda e: e.activation(qnT[:, :, :], pT[:, 512:768].rearrange("p (c q) -> p c q", c=2), AF.Copy), r=["B7"], w=["qnT"])
            for v in range(4):
                op("pe", lambda e, v=v: e.transpose(pT[:, v * 128:(v + 1) * 128], iqpad[:, v, :], ident[:, :]), r=["iqpad", "ident"], w=["B7"])
            op("act", lambda e: e.activation(iqTp[:, j, :, :], pT[:, 0:512].rearrange("p (v q) -> p v q", v=4), AF.Copy), r=["B7"], w=["iqTp%d" % j])
            for f in range(4):
                for rc in range(2):
                    op("pe", lambda e, f=f, rc=rc: e.matmul(pbk[3][:, f * 128:(f + 1) * 128], lhsT=w_uq_sb[:, rc, f * 128:(f + 1) * 128], rhs=qnT[:, rc, :],
                                                             start=(rc == 0), stop=(rc == 1)), r=["w_uq_sb", "qnT"], w=["B3"])
            op("act", lambda e: e.activation(qT[:, :, :], pbk[3][:, :].rearrange("p (c q) -> p c q", c=4), AF.Copy), r=["B3"], w=["qT"])
            for h in range(8):
                bk = 4 + h // 4
                op("pe", lambda e, h=h, bk=bk: e.matmul(pbk[bk][:, (h % 4) * 128:(h % 4 + 1) * 128], lhsT=w_ukTp[:, h, :], rhs=qT[:, h // 2, :],
                                                         start=True, stop=True), r=["w_ukTp", "qT"], w=[BK[bk]])
            op("act", lambda e: e.activation(qabsT[:, j, 0:4, :], pbk[4][:, :].rearrange("p (h q) -> p h q", h=4), AF.Copy), r=["B4"], w=["qabsT%d" % j])
            op("act", lambda e: e.activation(qabsT[:, j, 4:8, :], pbk[5][:, :].rearrange("p (h q) -> p h q", h=4), AF.Copy), r=["B5"], w=["qabsT%d" % j])

        def tile_B(s, t):
            j = (t - 1) % 4
            nk = t + 1
            N = nk * 128
            kbs = [t - 1, t]
            for ki, kb in enumerate(kbs):
                slot = kb % 6
                b0 = 2 * ki
                for g in range(2):
                    for half in range(2):
                        op("pe", lambda e, g=g, half=half, slot=slot, b0=b0: e.matmul(
                            pbk[b0 + g][:, half * 256:(half + 1) * 256], lhsT=akTp[:, slot, 2 * g + half, :],
                            rhs=aqT[:, j, 2 * g:2 * g + 2, :], start=True, stop=True),
                           r=["akTp", "aqT%d" % j], w=[BK[b0 + g]])
                for g in range(2):
                    op("act", lambda e, g=g, ki=ki, b0=b0: e.activation(eB[ki][:, 4 * g:4 * g + 4, :], pbk[b0 + g][:, :].rearrange("p (h q) -> p h q", h=4),
                                                                        AF.Exp, scale=0.125), r=[BK[b0 + g]], w=["eB%d" % ki])
                var = 0 if kb == t else (2 if kb == 0 else 1)
                op("dve", lambda e, ki=ki, var=var: e.tensor_tensor(pB[ki][:, :, :], eB[ki][:, :, :], EA[:, var, :, :], ALU.mult),
                   r=["eB%d" % ki, "EA"], w=["pB%d" % ki])
            for ki, kb in enumerate(kbs):
                slot = kb % 6
                for pos in range(8):
                    g = pos // 4
                    op("pe", lambda e, pos=pos, g=g, ki=ki, slot=slot: e.matmul(
                        pbk[4 + g][:, (pos % 4) * 65:(pos % 4) * 65 + 65], lhsT=pB[ki][:, pos, :], rhs=av1[:, slot, g, 0:65],
                        start=(ki == 0 and pos % 4 == 0), stop=(ki == 1 and pos % 4 == 3), skip_group_check=True), r=["pB%d" % ki, "av1"], w=[BK[4 + g]])
            for g in range(2):
                pv = pbk[4 + g][:, 0:260].rearrange("p (h d) -> p h d", h=4)
                op("dve", lambda e, g=g, pv=pv: e.tensor_tensor(stat2[:, 4 * g:4 * g + 4], pv[:, :, 64:65].rearrange("p h o -> p (h o)"),
                                                                esink[:, 4 * g:4 * g + 4], ALU.add), r=[BK[4 + g], "esink"], w=["stat2"])
            op("dve", lambda e: e.reciprocal(stat2[:, 0:8], stat2[:, 0:8]), r=["stat2"], w=["stat2"])
            for g in range(2):
                pv = pbk[4 + g][:, 0:260].rearrange("p (h d) -> p h d", h=4)
                op("dve", lambda e, g=g, pv=pv: e.tensor_tensor(oa_tok[:, g * 256:(g + 1) * 256].rearrange("p (h d) -> p h d", h=4), pv[:, :, 0:64],
                                                                stat2[:, 4 * g:4 * g + 4].unsqueeze(2).broadcast_to([128, 4, 64]), ALU.mult),
                   r=[BK[4 + g], "stat2"], w=["oa_tok"])
            for c in range(4):
                op("pe", lambda e, c=c: e.transpose(pT[:, c * 128:(c + 1) * 128], oa_tok[:, c * 128:(c + 1) * 128], ident[:, :]), r=["oa_tok", "ident"], w=["B7"])
            op("act", lambda e: e.activation(oaT[:, :, j * 128:(j + 1) * 128], pT[:, 0:512].rearrange("p (c q) -> p c q", c=4), AF.Copy),
               r=["B7"], w=["oaT%d" % j])
            grp = 0
            for k0 in range(0, N, 512):
                k1 = min(N, k0 + 512)
                for h in range(4):
                    bk = grp % 4
                    rb = grp % 2
                    grp += 1
                    op("pe", lambda e, h=h, bk=bk, k0=k0, k1=k1: e.matmul(pbk[bk][:, 0:k1 - k0], lhsT=iqTp[:, j, h, :], rhs=ikT[:, k0:k1], start=True, stop=True),
                       r=["iqTp%d" % j, "ikT"], w=[BK[bk]])
                    op("act", lambda e, bk=bk, rb=rb, k0=k0, k1=k1: e.activation(rbuf[rb][:, 0:k1 - k0], pbk[bk][:, 0:k1 - k0], AF.Relu), r=[BK[bk]], w=["rbuf%d" % rb])
                    if h == 0:
                        op("dve", lambda e, rb=rb, k0=k0, k1=k1: e.tensor_scalar(scores[:, k0:k1], rbuf[rb][:, 0:k1 - k0], iw_s[:, j, 0:1], None, ALU.mult),
                           r=["rbuf%d" % rb, "iw_s"], w=["scores"])
                    else:
                        op("dve", lambda e, rb=rb, k0=k0, k1=k1, h=h: e.scalar_tensor_tensor(scores[:, k0:k1], rbuf[rb][:, 0:k1 - k0], iw_s[:, j, h:h + 1],
                                                                                             scores[:, k0:k1], ALU.mult, ALU.add),
                           r=["rbuf%d" % rb, "iw_s", "scores"], w=["scores"])
            op("dve", lambda e: e.tensor_reduce(bis[:, 0:1], scores[:, 0:N], AX.X, ALU.max, apply_absolute_value=True), r=["scores"], w=["bisR"])
            negm = cst[:, C_NEGM:C_NEGM + 256].rearrange("p (v k) -> p v k", v=2)
            op("dve", lambda e: e.tensor_tensor(scores[:, t * 128:(t + 1) * 128], scores[:, t * 128:(t + 1) * 128], negm[:, 0, :], ALU.add), r=["scores", "cst"], w=["scores"])
            op("dve", lambda e: e.tensor_tensor(scores[:, 0:128], scores[:, 0:128], negm[:, 1, :], ALU.add), r=["scores", "cst"], w=["scores"])
            if t * 128 + 16 > topk:
                op("dve", lambda e: e.tensor_scalar(dtab[:, 0:n_iter], cst[:, C_CTAB:C_CTAB + n_iter], bis[:, 0:1], None, ALU.mult), r=["bisR", "cst"], w=["dtab"])
                op("dve", lambda e: e.tensor_scalar(dtab2n[:, 0:n_iter], dtab[:, 0:n_iter], -2.0, None, ALU.mult), r=["dtab"], w=["dtab2n"])
                op("dve", lambda e: e.memset(bis[:, 2:3], 0.0), r=[], w=["bisnm"])

        def bis_steps(t):
            N = (t + 1) * 128
            steps = []
            if t * 128 + 16 <= topk:
                return steps
            thr_s = float(2 * topk - N) - 0.5
            for it in range(n_iter):
                def step(it=it):
                    if it % 2 == 0:
                        op("act", lambda e: e.activation(mask[:, 0:N], scores[:, 0:N], AF.Sign, bias=bis[:, 2:3], accum_out=bis[:, 3:4]),
                           r=["scores", "bisnm"], w=["mask", "biscnt"])
                        op("dve", lambda e: e.scalar_tensor_tensor(bis[:, 4:5], bis[:, 3:4], thr_s, dtab2n[:, it:it + 1], ALU.is_ge, ALU.mult),
                           r=["biscnt", "dtab2n"], w=["bisu"])
                    else:
                        op("dve", lambda e: e.tensor_scalar(bis[:, 5:6], bis[:, 2:3], -1.0, None, ALU.mult), r=["bisnm"], w=["bismid"])
                        op("dve", lambda e: e.tensor_scalar(mask[:, 0:N], scores[:, 0:N], bis[:, 5:6], None, ALU.is_ge, ALU.add, accum_out=bis[:, 3:4]),
                           r=["scores", "bismid"], w=["mask", "biscnt"])
                        op("dve", lambda e: e.scalar_tensor_tensor(bis[:, 4:5], bis[:, 3:4], float(topk) - 0.5, dtab2n[:, it:it + 1], ALU.is_ge, ALU.mult),
                           r=["biscnt", "dtab2n"], w=["bisu"])
                    op("dve", lambda e: e.scalar_tensor_tensor(bis[:, 2:3], bis[:, 4:5], bis[:, 2:3], dtab[:, it:it + 1], ALU.add, ALU.add),
                       r=["bisu", "bisnm", "dtab"], w=["bisnm"])
                steps.append(step)
            return steps

        def tile_B1post(s, t):
            j = (t - 1) % 4
            par = j % 2
            nk = t + 1
            N = nk * 128
            if t * 128 + 16 <= topk:
                op("dve", lambda e: e.memset(bis[:, 1:2], -1e29), r=[], w=["bisthr"])
            else:
                op("dve", lambda e: e.scalar_tensor_tensor(bis[:, 1:2], bis[:, 2:3], -1.0, dtab[:, n_iter - 1:n_iter], ALU.mult, ALU.subtract),
                   r=["bisnm", "dtab"], w=["bisthr"])
            op("dve", lambda e: e.tensor_scalar(mask[:, 0:N], scores[:, 0:N], bis[:, 1:2], None, ALU.is_ge), r=["scores", "bisthr"], w=["mask"])
            for k0 in range(0, nk, 8):
                k1 = min(nk, k0 + 8)
                for kc in range(k0, k1):
                    op("pe", lambda e, kc=kc, k0=k0: e.transpose(pT[:, (kc - k0) * 128:(kc - k0 + 1) * 128], mask[:, kc * 128:(kc + 1) * 128], ident[:, :]),
                       r=["mask", "ident"], w=["B7"])
                op("act", lambda e, k0=k0, k1=k1: e.activation(maskT[par][:, k0:k1, :], pT[:, 0:(k1 - k0) * 128].rearrange("p (k q) -> p k q", q=128), AF.Copy),
                   r=["B7"], w=["maskT%d" % par])

        def tile_B2(s, t, steps):
            j = (t - 1) % 4
            par = j % 2
            nk = t + 1
            for kc in range(nk):
                pp = kc % 2
                b0 = 2 * pp
                for hh in range(2):
                    op("pe", lambda e, kc=kc, hh=hh, b0=b0: e.matmul(pbk[b0 + hh][:, :], lhsT=ckvT[:, kc * 128:(kc + 1) * 128],
                                                                     rhs=qabsT[:, j, 4 * hh:4 * hh + 4, :], start=True, stop=True),
                       r=["ckvT", "qabsT%d" % j], w=[BK[b0 + hh]])
                    op("act", lambda e, hh=hh, pp=pp, b0=b0: e.activation(eB[pp][:, 4 * hh:4 * hh + 4, :], pbk[b0 + hh][:, :].rearrange("p (h q) -> p h q", h=4),
                                                                           AF.Exp, scale=0.125), r=[BK[b0 + hh]], w=["eB%d" % pp])
                if kc >= t - 1:
                    vi = 0 if kc == t else 1
                    op("dve", lambda e, pp=pp, vi=vi: e.tensor_tensor(eB[pp][:, :, :], eB[pp][:, :, :], EB[:, vi, :, :], ALU.mult),
                       r=["eB%d" % pp, "EB"], w=["eB%d" % pp])
                op("dve", lambda e, pp=pp, kc=kc: e.tensor_tensor(pB[pp][:, :, :], eB[pp][:, :, :], maskT[par][:, kc:kc + 1, :].broadcast_to([128, 8, 128]), ALU.mult),
                   r=["eB%d" % pp, "maskT%d" % par], w=["pB%d" % pp])
                for h in range(8):
                    bk = 4 + h // 3
                    o0 = (h % 3) * 129
                    op("pe", lambda e, h=h, bk=bk, o0=o0, pp=pp, kc=kc: e.matmul(pbk[bk][:, o0:o0 + 129], lhsT=pB[pp][:, h, :], rhs=ckv1[:, kc, 0:129],
                                                                                 start=(kc == 0 and h % 3 == 0), stop=(kc == nk - 1 and (h % 3 == 2 or h == 7)),
                                                                                 skip_group_check=True),
                       r=["pB%d" % pp, "ckv1"], w=[BK[bk]])
                if steps:
                    steps.pop(0)()
            while steps:
                steps.pop(0)()
            for b in range(3):
                nh = 3 if b < 2 else 2
                pv = pbk[4 + b][:, 0:nh * 129].rearrange("p (h d) -> p h d", h=nh)
                op("act", lambda e, b=b, nh=nh, pv=pv: e.activation(stat2[:, 8 + 3 * b:8 + 3 * b + nh], pv[:, :, 128:129].rearrange("p h o -> p (h o)"), AF.Copy),
                   r=[BK[4 + b]], w=["stat2"])
                op("dve", lambda e, b=b, nh=nh: e.reciprocal(stat2[:, 8 + 3 * b:8 + 3 * b + nh], stat2[:, 8 + 3 * b:8 + 3 * b + nh]), r=["stat2"], w=["stat2"])
                op("dve", lambda e, b=b, nh=nh, pv=pv: e.tensor_tensor(lat[:, 3 * b:3 * b + nh, :], pv[:, :, 0:128],
                                                                       stat2[:, 8 + 3 * b:8 + 3 * b + nh].unsqueeze(2).broadcast_to([128, nh, 128]), ALU.mult),
                   r=[BK[4 + b], "stat2"], w=["lat"])
            for h in range(8):
                op("pe", lambda e, h=h: e.transpose(pT[:, h * 128:(h + 1) * 128], lat[:, h, :], ident[:, :]), r=["lat", "ident"], w=["B7"])
            op("act", lambda e: e.activation(latT[:, :, :], pT[:, :].rearrange("p (h q) -> p h q", h=8), AF.Copy), r=["B7"], w=["latT"])
            for h in range(8):
                op("pe", lambda e, h=h: e.matmul(pbk[0][:, (h // 2) * 128:(h // 2 + 1) * 128], lhsT=w_uvp[:, h, :], rhs=latT[:, h, :],
                                                  start=(h % 2 == 0), stop=(h % 2 == 1)), r=["w_uvp", "latT"], w=["B0"])
            op("act", lambda e: e.activation(obT[:, :, j * 128:(j + 1) * 128], pbk[0][:, :].rearrange("p (c q) -> p c q", c=4), AF.Copy),
               r=["B0"], w=["obT%d" % j])

        def phase_C(s, b):
            for c in range(8):
                sl = c % 2
                skc = ["sq0", "sq1", "sq2"] if sl == 0 else ["sq3", "sq4", "sq5"]
                dma("sp", stream[:, sl, :], pc_s[c], r=["scr"], w=skc, sem="s_st%d" % sl)
                U = stream[:, sl, :]
                b0 = 4 * sl if sl == 0 else 3
                banks = [0, 1, 2, 3] if sl == 0 else [4, 5, 6, 3]
                for kc in range(4):
                    op("pe", lambda e, kc=kc, U=U, bk=banks[0]: e.matmul(pbk[bk][:, :], lhsT=U[:, kc * 128:(kc + 1) * 128], rhs=oaT[:, kc, :], start=(kc == 0), stop=(kc == 3)),
                       r=skc + ["oaT%d" % q for q in range(4)], w=[BK[banks[0]]])
                for kc in range(4):
                    op("pe", lambda e, kc=kc, U=U, bk=banks[1]: e.matmul(pbk[bk][:, :], lhsT=U[:, 512 + kc * 128:512 + (kc + 1) * 128], rhs=obT[:, kc, :], start=(kc == 0), stop=(kc == 3)),
                       r=skc + ["obT%d" % q for q in range(4)], w=[BK[banks[1]]])
                for ab in range(2):
                    for kc in range(8):
                        op("pe", lambda e, kc=kc, U=U, ab=ab, bk=banks[2 + ab]: e.matmul(pbk[bk][:, :], lhsT=U[:, 1024 + ab * 1024 + kc * 128:1024 + ab * 1024 + (kc + 1) * 128],
                                                                                         rhs=hnT[:, kc, :], start=(kc == 0), stop=(kc == 7)),
                           r=skc + ["hnT%d" % q for q in range(4)], w=[BK[banks[2 + ab]]])
                    op("act", lambda e, ab=ab, sl=sl, bk=banks[2 + ab], c=c: e.activation(tAB[sl][:, ab, :], pbk[bk][:, :], AF.Tanh, scale=0.5, bias=hbg[:, ab * 8 + c:ab * 8 + c + 1]),
                       r=[BK[banks[2 + ab]], "hbg"], w=["tAB%d%d" % (sl, ab)])
                    op("dve", lambda e, ab=ab, sl=sl, bk=banks[ab]: e.scalar_tensor_tensor(mAB[sl][:, ab, :], tAB[sl][:, ab, :], 1.0, pbk[bk][:, :], ALU.add, ALU.mult),
                       r=["tAB%d%d" % (sl, ab), BK[banks[ab]]], w=["mAB%d%d" % (sl, ab)])
                op("pool", lambda e, sl=sl, c=c: e.tensor_tensor(mixedT[:, c, :], mAB[sl][:, 0, :], mAB[sl][:, 1, :], ALU.add),
                   r=["mAB%d0" % sl, "mAB%d1" % sl], w=["mixedT"])
            for j in range(4):
                for half in range(2):
                    bk = half
                    for c in range(8):
                        op("pe", lambda e, c=c, j=j, half=half, bk=bk: e.matmul(pbk[bk][:, :], lhsT=mixedT[:, c, j * 128:(j + 1) * 128],
                                                                                rhs=w_out_sb[:, c, half * 512:(half + 1) * 512], start=(c == 0), stop=(c == 7)),
                           r=["mixedT", "w_out_sb"], w=[BK[bk]])
                    op("dve", lambda e, j=j, half=half, bk=bk: e.scalar_tensor_tensor(xh[:, j, half * 512:(half + 1) * 512], pbk[bk][:, :], 0.5,
                                                                                      xh[:, j, half * 512:(half + 1) * 512], ALU.mult, ALU.add),
                       r=[BK[bk], "xh%d" % j], w=["xh%d" % j])
                op("act", lambda e, j=j: e.activation(junk[:, :], xh[:, j, :], AF.Square, accum_out=stat[:, 16 + j:17 + j]), r=["xh%d" % j], w=["junk", "stat"])
            rstd_from(stat[:, 16:20], stat[:, 16:20], 1.0 / D, ["stat"])
            for j in range(4):
                norm_T(xh[:, j, :], stat[:, 16 + j:17 + j], C_GFFN, hn2T[:, :, j * 128:(j + 1) * 128], ["xh%d" % j, "stat"], ["hn2T"])

        def phase_DE(s, b):
            sflat = stream[:, :, :].rearrange("p a e -> p (a e)")
            sl3 = [sflat[:, k * FF_UNIT:(k + 1) * FF_UNIT] for k in range(3)]
            sk3 = [["sq0", "sq1"], ["sq2", "sq3"], ["sq4", "sq5"]]
            for q in range(2):
                dma("sp", wdb[q][:, :, :], wd_s[q].rearrange("p (k f) -> p k f", k=NFC), r=["scr"], w=["wdb%d" % q], sem="s_wd%d" % q)
            for fc in range(NFC):
                sl = fc % 3
                pb2 = fc % 2
                dma("sp", sl3[sl], ff_s[fc], r=["scr"], w=sk3[sl], sem="s_sf%d" % sl)
                V = sl3[sl]
                bg, bu = 2 * pb2, 2 * pb2 + 1
                for kc in range(8):
                    op("pe", lambda e, kc=kc, V=V, bg=bg: e.matmul(pbk[bg][:, :], lhsT=V[:, kc * 128:(kc + 1) * 128], rhs=hn2T[:, kc, :], start=(kc == 0), stop=(kc == 7)),
                       r=sk3[sl] + ["hn2T"], w=[BK[bg]])
                for kc in range(8):
                    op("pe", lambda e, kc=kc, V=V, bu=bu: e.matmul(pbk[bu][:, :], lhsT=V[:, 1024 + kc * 128:1024 + (kc + 1) * 128], rhs=hn2T[:, kc, :], start=(kc == 0), stop=(kc == 7)),
                       r=sk3[sl] + ["hn2T"], w=[BK[bu]])
                op("act", lambda e, pb2=pb2, bg=bg: e.activation(tg[pb2], pbk[bg][:, :], AF.Tanh, scale=0.5), r=[BK[bg]], w=["tAB%d0" % pb2])
                op("dve", lambda e, pb2=pb2, bg=bg: e.scalar_tensor_tensor(a1[pb2], tg[pb2], 1.0, pbk[bg][:, :], ALU.add, ALU.mult), r=["tAB%d0" % pb2, BK[bg]], w=["mAB%d0" % pb2])
                op("dve", lambda e, pb2=pb2, bu=bu, fc=fc: e.tensor_tensor(actT[:, fc, :], a1[pb2], pbk[bu][:, :], ALU.mult), r=["mAB%d0" % pb2, BK[bu]], w=["actT"])
            for q in range(8):
                ws = q % 2
                if q >= 2:
                    dma("sp", wdb[ws][:, :, :], wd_s[q].rearrange("p (k f) -> p k f", k=NFC), r=["scr"], w=["wdb%d" % ws], sem="s_wd%d" % ws)
                for j in range(4):
                    bk = 4 + (q * 4 + j) % 3
                    for fc in range(NFC):
                        op("pe", lambda e, fc=fc, j=j, ws=ws, bk=bk: e.matmul(pbk[bk][:, 0:128], lhsT=actT[:, fc, j * 128:(j + 1) * 128], rhs=wdb[ws][:, fc, :],
                                                                              start=(fc == 0), stop=(fc == NFC - 1)), r=["actT", "wdb%d" % ws], w=[BK[bk]])
                    op("dve", lambda e, j=j, q=q, bk=bk: e.scalar_tensor_tensor(xh[:, j, q * 128:(q + 1) * 128], pbk[bk][:, 0:128], 0.5,
                                                                                 xh[:, j, q * 128:(q + 1) * 128], ALU.mult, ALU.add),
                       r=[BK[bk], "xh%d" % j], w=["xh%d" % j])
            for j in range(4):
                op("act", lambda e, j=j: e.activation(junk[:, :], xh[:, j, :], AF.Square, accum_out=stat[:, 24 + j:25 + j]), r=["xh%d" % j], w=["junk", "stat"])
            rstd_from(stat[:, 24:28], stat[:, 24:28], 1.0 / D, ["stat"])
            for j in range(4):
                t = 4 * b + j + 1
                eng = "dve" if j % 2 == 0 else "pool"
                if eng == "dve":
                    op("dve", lambda e, j=j: e.scalar_tensor_tensor(xh[:, j, :], xh[:, j, :], stat[:, 24 + j:25 + j], cst[:, C_GFIN:C_GFIN + D], ALU.mult, ALU.mult),
                       r=["xh%d" % j, "stat", "cst"], w=["xh%d" % j])
                else:
                    op("pool", lambda e, j=j: e.tensor_scalar(xh[:, j, :], xh[:, j, :], stat[:, 24 + j:25 + j], None, ALU.mult), r=["xh%d" % j, "stat"], w=["xh%d" % j])
                    op("pool", lambda e, j=j: e.tensor_tensor(xh[:, j, :], xh[:, j, :], cst[:, C_GFIN:C_GFIN + D], ALU.mult), r=["xh%d" % j, "cst"], w=["xh%d" % j])
                dma("sp", y_d[s, (t - 1) * 128:t * 128, :], xh[:, j, :], r=["xh%d" % j], w=["y"], sem="s_x%d" % j)

        for s in range(nseq):
            try:
                tile_A(s, 0)
            except _Stop:
                P.finish([])
                return nc
            if stop == 4:
                P.finish([])
                return nc
            for b in range(nblk // 4):
                tl = [4 * b + j + 1 for j in range(4)]
                P.phase = "A"
                tile_A(s, tl[0])
                P.phase = "B"
                tile_B(s, tl[0])
                pend.extend(bis_steps(tl[0]))
                P.phase = "A"
                tile_A(s, tl[1])
                P.phase = "B"
                while pend:
                    pend.pop(0)()
                tile_B1post(s, tl[0])
                for ii in range(1, 4):
                    if ii + 1 < 4:
                        P.phase = "A"
                        tile_A(s, tl[ii + 1])
                        P.phase = "B"
                    tile_B(s, tl[ii])
                    tile_B2(s, tl[ii - 1], bis_steps(tl[ii]))
                    tile_B1post(s, tl[ii])
                tile_B2(s, tl[3], [])
                P.barrier()
                P.phase = "C"
                phase_C(s, b)
                P.phase = "DE"
                if stop == 8:
                    P.finish([])
                    return nc
                phase_DE(s, b)
                P.barrier()
        P.finish(["s_x0", "s_x1", "s_x2", "s_x3"])
    return nc


def _host_consts(inp):
    f32 = np.float32
    cst = np.zeros((128, NCONST), f32)
    cst[:, C_GATT:C_GATT + 8] = np.asarray(inp["attn_norm_g"], f32)[0].reshape(8, 128).T
    cst[:, C_GFFN:C_GFFN + 8] = np.asarray(inp["ffn_norm_g"], f32)[0].reshape(8, 128).T
    cst[:, C_BG:C_BG + 16] = np.asarray(inp["b_gates"], f32)[0].reshape(16, 128).T
    cst[:, C_SINK:C_SINK + 8] = np.asarray(inp["sinks"], f32)[0][HEADPOS][None, :]
    rb = np.asarray(inp["rel_bias"], f32)
    cst[:, C_BFAR:C_BFAR + 8] = rb[31, 8:16][None, :]
    cst[:, C_GQ:C_GQ + 256] = np.asarray(inp["q_norm_g"], f32)[0][None, :]
    cst[:, C_GKV:C_GKV + 128] = np.asarray(inp["kv_norm_g"], f32)[0][None, :]
    cst[:, C_LNG:C_LNG + 64] = np.asarray(inp["idx_k_ln_g"], f32)[0][None, :]
    cst[:, C_LNB:C_LNB + 64] = np.asarray(inp["idx_k_ln_b"], f32)[0][None, :]
    cst[:, C_GFIN:C_GFIN + D] = np.asarray(inp["final_norm_g"], f32)[None, :]
    qi = np.arange(128)[:, None]
    ki = np.arange(128)[None, :]
    negm = np.zeros((128, 2, 128), f32)
    negm[:, 0, :] = np.where(ki <= qi, 0.0, -1e30)
    negm[:, 1, :] = np.where(ki >= NPAD, 0.0, -1e30)
    cst[:, C_NEGM:C_NEGM + 256] = negm.reshape(128, 256)
    kk = np.arange(128)[:, None]
    qq = np.arange(128)[None, :]
    maskA = np.zeros((128, 3, 128), f32)
    maskA[:, 0, :] = (qq >= kk)
    maskA[:, 1, :] = (qq < kk)
    maskA[:, 2, :] = (qq < kk) & (kk >= NPAD)
    cst[:, C_MASKA:C_MASKA + 384] = maskA.reshape(128, 384)
    cst[:, C_CTAB:C_CTAB + 16] = (0.5 ** np.arange(1, 17, dtype=np.float64)).astype(f32)[None, :]
    dist_d = np.maximum(qq - kk, 0)
    dist_s = 128 + qq - kk
    bd = _t5_bucket_np(dist_d)
    bs = _t5_bucket_np(dist_s)
    biasT = np.zeros((128, 4, 8, 128), f32)
    for pos in range(8):
        biasT[:, 0, pos, :] = rb[bd, HEADPOS[pos]]
        biasT[:, 1, pos, :] = rb[bs, HEADPOS[pos]]
        biasT[:, 2, pos, :] = rb[bd, 8 + pos]
        biasT[:, 3, pos, :] = rb[bs, 8 + pos]
    return cst, biasT.reshape(128, 4 * 8 * 128)


def _in_maps(inp, ncores, nseq):
    f32 = np.float32
    cst, biasT = _host_consts(inp)
    wa = np.asarray(inp["w_branch_a"], f32)[0].reshape(8, 64, D)[HEADPOS].reshape(512, D)
    common = {
        "meta": np.ascontiguousarray(np.asarray(inp["meta_tokens"], f32)),
        "w_in": np.ascontiguousarray(np.asarray(inp["w_in"], f32)[0]),
        "w_uq": np.ascontiguousarray(np.asarray(inp["w_uq"], f32)[0]),
        "w_uk": np.ascontiguousarray(np.asarray(inp["w_uk"], f32)[0].reshape(128, 512)),
        "w_uv": np.ascontiguousarray(np.asarray(inp["w_uv"], f32)[0].reshape(128, 512)),
        "w_a": np.ascontiguousarray(wa),
        "w_b": np.ascontiguousarray(np.asarray(inp["w_branch_b"], f32)[0]),
        "w_out": np.ascontiguousarray(np.asarray(inp["w_out"], f32)[0]),
        "wg": np.ascontiguousarray(np.asarray(inp["w_ffn_gate"], f32)[0]),
        "wu": np.ascontiguousarray(np.asarray(inp["w_ffn_up"], f32)[0]),
        "wd": np.ascontiguousarray(np.asarray(inp["w_ffn_down"], f32)[0]),
        "cst": cst, "biasT": biasT, "ident": np.eye(128, dtype=f32),
    }
    x = np.asarray(inp["x"], f32)
    maps = []
    for c in range(ncores):
        m = dict(common)
        m["x"] = np.ascontiguousarray(x[c * nseq:(c + 1) * nseq])
        maps.append(m)
    return maps


def kernel(**inputs):
    ncores = 8
    x = inputs["x"]
    bsz, seq, _ = x.shape
    nseq = bsz // ncores
    nblk = seq // 128
    topk = min(256, seq // 4)
    nc = build_nc(nseq, nblk, topk)
    maps = _in_maps(inputs, ncores, nseq)
    res = run_bass_kernel_spmd(nc, maps, core_ids=list(range(ncores)))
    out = np.concatenate([np.asarray(r["y"], np.float32) for r in res.results], axis=0)
    return out
```
